# Optimizing a Trainium2 kernel written in Bass

```python
import jax, jax.numpy as jnp
from jax import lax
import numpy as np

D_MODEL = 1024
BATCH = 8
SEQ = 4096
DEPTH = 2

N_EVEN = (DEPTH + 1) // 2
N_ODD = DEPTH // 2
PLE_DIM = 256
LN_EPS = 1e-5
RMS_EPS = 1e-5
DEEPNORM_ALPHA = (2.0 * DEPTH) ** 0.25
DEEPNORM_BETA = (8.0 * DEPTH) ** -0.25

FOX_HEAD_DIM = 64
FOX_HEADS = D_MODEL // FOX_HEAD_DIM
FOX_Q_BLOCK = 128
FOX_IN = 3 * D_MODEL + FOX_HEADS

SSD_EXPAND = 2
SSD_D_INNER = SSD_EXPAND * D_MODEL
SSD_HEAD_DIM = 64
SSD_HEADS = SSD_D_INNER // SSD_HEAD_DIM
SSD_GROUPS = 8
SSD_HEADS_PER_GROUP = SSD_HEADS // SSD_GROUPS
SSD_STATE = 128
SSD_CONV = 4
SSD_CHUNK = 128
SSD_CONV_CH = SSD_D_INNER + 2 * SSD_GROUPS * SSD_STATE
SSD_IN = SSD_D_INNER + SSD_CONV_CH + SSD_HEADS

FFN_DIM = 2816
N_EXPERTS = 8
TOP_K = 2
EXPERT_DIM = 3584

kernel_name = 'fox_ssd_interleaved_deepnorm_moe'


def layer_norm(x, g, b):
    xf = x.astype(jnp.float32)
    mu = jnp.mean(xf, axis=-1, keepdims=True)
    var = jnp.mean(jnp.square(xf - mu), axis=-1, keepdims=True)
    return ((xf - mu) * lax.rsqrt(var + LN_EPS) * g + b).astype(x.dtype)


def forgetting_attention(x, w_in, b_f, w_o):
    bsz, seq, _ = x.shape
    proj = x @ w_in
    q, k, v, f_logit = jnp.split(proj, [D_MODEL, 2 * D_MODEL, 3 * D_MODEL], axis=-1)

    def heads(t):
        return t.reshape(bsz, seq, FOX_HEADS, FOX_HEAD_DIM).transpose(0, 2, 1, 3)

    q, k, v = heads(q), heads(k), heads(v)
    log_f = jax.nn.log_sigmoid((f_logit + b_f).astype(jnp.float32))
    cum = jnp.cumsum(log_f, axis=1).transpose(0, 2, 1)
    scale = FOX_HEAD_DIM ** -0.5
    key_pos = jnp.arange(seq)

    def q_block(start):
        qb = lax.dynamic_slice_in_dim(q, start, FOX_Q_BLOCK, axis=2)
        cb = lax.dynamic_slice_in_dim(cum, start, FOX_Q_BLOCK, axis=2)
        logits = jnp.einsum('bhqd,bhkd->bhqk', qb, k).astype(jnp.float32) * scale
        logits = logits + cb[..., :, None] - cum[..., None, :]
        q_pos = start + jnp.arange(FOX_Q_BLOCK)
        causal = key_pos[None, :] <= q_pos[:, None]
        logits = jnp.where(causal, logits, -jnp.inf)
        probs = jax.nn.softmax(logits, axis=-1).astype(v.dtype)
        return jnp.einsum('bhqk,bhkd->bhqd', probs, v)

    starts = jnp.arange(0, seq, FOX_Q_BLOCK)
    out = lax.map(q_block, starts)
    out = out.transpose(1, 0, 3, 2, 4).reshape(bsz, seq, D_MODEL)
    return out @ w_o


def causal_depthwise_conv(u, w, b):
    out = lax.conv_general_dilated(
        u, w[:, None, :].astype(u.dtype), window_strides=(1,), padding=[(SSD_CONV - 1, 0)],
        dimension_numbers=('NWC', 'WIO', 'NWC'), feature_group_count=u.shape[-1])
    return out + b


def ssd_mixer(x, w_in, conv_w, conv_b, dt_bias, a_log, d_skip, norm_g, w_out):
    bsz, seq, _ = x.shape
    G, R, P, N, L = SSD_GROUPS, SSD_HEADS_PER_GROUP, SSD_HEAD_DIM, SSD_STATE, SSD_CHUNK
    nc = seq // L
    f32 = jnp.float32
    proj = x @ w_in
    z, xbc, dt_raw = jnp.split(proj, [SSD_D_INNER, SSD_D_INNER + SSD_CONV_CH], axis=-1)
    xbc = jax.nn.silu(causal_depthwise_conv(xbc, conv_w, conv_b))
    xs, bm, cm = jnp.split(xbc, [SSD_D_INNER, SSD_D_INNER + G * N], axis=-1)
    xs = xs.astype(f32).reshape(bsz, nc, L, G, R, P)
    bm = bm.astype(f32).reshape(bsz, nc, L, G, N)
    cm = cm.astype(f32).reshape(bsz, nc, L, G, N)
    dt = jax.nn.softplus(dt_raw.astype(f32) + dt_bias.astype(f32)).reshape(bsz, nc, L, G, R)
    a = -jnp.exp(a_log.astype(f32)).reshape(G, R)
    da_cs = jnp.cumsum(dt * a, axis=2)
    xdt = xs * dt[..., None]
    seg = da_cs[:, :, :, None] - da_cs[:, :, None, :]
    causal = jnp.tril(jnp.ones((L, L), dtype=bool))[:, :, None, None]
    decay = jnp.exp(jnp.where(causal, seg, -jnp.inf))
    cb = jnp.einsum('bclgn,bcsgn->bclsg', cm, bm)
    y_diag = jnp.einsum('bclsgr,bcsgrp->bclgrp', cb[..., None] * decay, xdt)
    decay_to_end = jnp.exp(da_cs[:, :, -1:] - da_cs)
    states = jnp.einsum('bclgn,bclgrp->bcgrpn', bm, xdt * decay_to_end[..., None])
    chunk_decay = jnp.exp(da_cs[:, :, -1])

    def step(h, inp):
        s_c, a_c = inp
        return a_c[..., None, None] * h + s_c, h

    h0 = jnp.zeros((bsz, G, R, P, N), f32)
    _, h_in = lax.scan(step, h0, (states.transpose(1, 0, 2, 3, 4, 5), chunk_decay.transpose(1, 0, 2, 3)))
    h_in = h_in.transpose(1, 0, 2, 3, 4, 5)
    y_off = jnp.einsum('bclgn,bcgrpn->bclgrp', cm, h_in) * jnp.exp(da_cs)[..., None]
    y = y_diag + y_off + d_skip.astype(f32).reshape(G, R, 1) * xs
    y = y.reshape(bsz, seq, SSD_D_INNER)
    y = y * jax.nn.silu(z.astype(f32))
    yg = y.reshape(bsz, seq, G, SSD_D_INNER // G)
    yg = yg * lax.rsqrt(jnp.mean(jnp.square(yg), axis=-1, keepdims=True) + RMS_EPS)
    y = (yg.reshape(bsz, seq, SSD_D_INNER) * norm_g).astype(x.dtype)
    return y @ w_out


def swiglu(x, w_gate, w_up, w_down):
    return (jax.nn.silu(x @ w_gate) * (x @ w_up)) @ w_down


def moe_swiglu(x, router, w_gate, w_up, w_down):
    logits = (x @ router).astype(jnp.float32)
    top_vals, top_idx = lax.top_k(logits, TOP_K)
    top_w = jax.nn.softmax(top_vals, axis=-1)
    gates = jnp.sum(jax.nn.one_hot(top_idx, N_EXPERTS, dtype=jnp.float32) * top_w[..., None], axis=-2)
    out = jnp.zeros_like(x)
    for e in range(N_EXPERTS):
        y_e = swiglu(x, w_gate[e], w_up[e], w_down[e])
        out = out + (gates[..., e:e + 1] * y_e).astype(x.dtype)
    return out


def setup_inputs(seed: int = 0) -> dict:
    key = jax.random.key(seed)
    ks = iter(jax.random.split(key, 40))
    f32 = jnp.float32

    def normal(shape, scale):
        return jax.random.normal(next(ks), shape, f32) * scale

    def gain(shape):
        return 1.0 + normal(shape, 0.02)

    beta = DEEPNORM_BETA
    dt0 = jnp.exp(jax.random.uniform(next(ks), (N_ODD, SSD_HEADS), f32, np.log(1e-3), np.log(1e-1)))
    dt_bias = dt0 + jnp.log(-jnp.expm1(-dt0))
    a_log = jnp.log(jax.random.uniform(next(ks), (N_ODD, SSD_HEADS), f32, 1.0, 16.0))
    fox_b_f = jax.random.uniform(next(ks), (N_EVEN, FOX_HEADS), f32, 1.0, 6.0)
    return {
        'x': normal((BATCH, SEQ, D_MODEL), 1.0),
        'p': normal((DEPTH, BATCH, SEQ, PLE_DIM), 1.0),
        'ln_mix_g': gain((DEPTH, D_MODEL)),
        'ln_mix_b': normal((DEPTH, D_MODEL), 0.02),
        'ln_ffn_g': gain((DEPTH, D_MODEL)),
        'ln_ffn_b': normal((DEPTH, D_MODEL), 0.02),
        'fox_w_in': normal((N_EVEN, D_MODEL, FOX_IN), D_MODEL ** -0.5),
        'fox_b_f': fox_b_f,
        'fox_w_o': normal((N_EVEN, D_MODEL, D_MODEL), beta * D_MODEL ** -0.5),
        'ssd_w_in': normal((N_ODD, D_MODEL, SSD_IN), D_MODEL ** -0.5),
        'ssd_conv_w': normal((N_ODD, SSD_CONV, SSD_CONV_CH), SSD_CONV ** -0.5),
        'ssd_conv_b': normal((N_ODD, SSD_CONV_CH), 0.02),
        'ssd_dt_bias': dt_bias,
        'ssd_a_log': a_log,
        'ssd_d': gain((N_ODD, SSD_HEADS)),
        'ssd_norm_g': gain((N_ODD, SSD_D_INNER)),
        'ssd_w_out': normal((N_ODD, SSD_D_INNER, D_MODEL), beta * SSD_D_INNER ** -0.5),
        'ffn_w_gate': normal((N_EVEN, D_MODEL, FFN_DIM), D_MODEL ** -0.5),
        'ffn_w_up': normal((N_EVEN, D_MODEL, FFN_DIM), D_MODEL ** -0.5),
        'ffn_w_down': normal((N_EVEN, FFN_DIM, D_MODEL), beta * FFN_DIM ** -0.5),
        'moe_router': normal((N_ODD, D_MODEL, N_EXPERTS), D_MODEL ** -0.5),
        'moe_w_gate': normal((N_ODD, N_EXPERTS, D_MODEL, EXPERT_DIM), D_MODEL ** -0.5),
        'moe_w_up': normal((N_ODD, N_EXPERTS, D_MODEL, EXPERT_DIM), D_MODEL ** -0.5),
        'moe_w_down': normal((N_ODD, N_EXPERTS, EXPERT_DIM, D_MODEL), beta * EXPERT_DIM ** -0.5),
        'ple_w_proj': normal((DEPTH, PLE_DIM, D_MODEL), PLE_DIM ** -0.5),
        'ple_w_gate': normal((DEPTH, D_MODEL, D_MODEL), D_MODEL ** -0.5),
    }


def reference(x, p, ln_mix_g, ln_mix_b, ln_ffn_g, ln_ffn_b,
              fox_w_in, fox_b_f, fox_w_o,
              ssd_w_in, ssd_conv_w, ssd_conv_b, ssd_dt_bias, ssd_a_log, ssd_d, ssd_norm_g, ssd_w_out,
              ffn_w_gate, ffn_w_up, ffn_w_down,
              moe_router, moe_w_gate, moe_w_up, moe_w_down,
              ple_w_proj, ple_w_gate):
    h = x
    for i in range(DEPTH):
        j = i // 2
        if i % 2 == 0:
            mix = forgetting_attention(h, fox_w_in[j], fox_b_f[j], fox_w_o[j])
        else:
            mix = ssd_mixer(h, ssd_w_in[j], ssd_conv_w[j], ssd_conv_b[j], ssd_dt_bias[j],
                            ssd_a_log[j], ssd_d[j], ssd_norm_g[j], ssd_w_out[j])
        h = layer_norm(DEEPNORM_ALPHA * h + mix, ln_mix_g[i], ln_mix_b[i])
        if i % 2 == 0:
            ffn = swiglu(h, ffn_w_gate[j], ffn_w_up[j], ffn_w_down[j])
        else:
            ffn = moe_swiglu(h, moe_router[j], moe_w_gate[j], moe_w_up[j], moe_w_down[j])
        h = layer_norm(DEEPNORM_ALPHA * h + ffn, ln_ffn_g[i], ln_ffn_b[i])
        h = h + (p[i] @ ple_w_proj[i]) * jax.nn.sigmoid(h @ ple_w_gate[i])
    return h
```

```python
import numpy as np
from contextlib import ExitStack
import concourse.bass as bass
import concourse.mybir as mybir
from concourse.bass_utils import run_bass_kernel_spmd

F32 = mybir.dt.float32
BF16 = mybir.dt.bfloat16
AF = mybir.ActivationFunctionType
ALU = mybir.AluOpType
AX = mybir.AxisListType

S = 4096
D = 1024
NT = S // 128
ALPHA = (2.0 * 2) ** 0.25
LN_EPS = 1e-5
RMS_EPS = 1e-5
FFN = 2816
NE = 8
EDIM = 3584
COMPUTE = ("pe", "act", "dve", "pool")


class _Op:
    __slots__ = ("eng", "fn", "is_dma", "stream", "waits", "signal", "sig_sem", "sig_val", "idx")

    def __init__(self, eng, fn, is_dma, stream):
        self.eng = eng
        self.fn = fn
        self.is_dma = is_dma
        self.stream = stream
        self.waits = []
        self.signal = is_dma
        self.sig_sem = None
        self.sig_val = 0


class Prog:
    def __init__(self, nc, stack, nstream_sems=6):
        self.nc = nc
        self.stack = stack
        self.ops = []
        self.emitted = 0
        self.lastw = {}
        self.readers = {}
        self.engs = {"pe": nc.tensor, "act": nc.scalar, "dve": nc.vector,
                     "pool": nc.gpsimd, "sp": nc.sync}
        self.sem = {e: stack.enter_context(nc.semaphore("s_" + e)) for e in COMPUTE}
        self.cnt = {e: 0 for e in COMPUTE}
        self.st_sems = {}
        self.st_cnt = {}
        self.waited = {e: {} for e in self.engs}
        self.K = nstream_sems
        self.n_inst = {e: 0 for e in self.engs}
        self._grp = None
        self._cur = None

    def begin_group(self):
        self._grp = []
        self._cur = None

    def next_stream(self):
        self._cur = []
        self._grp.append(self._cur)

    def end_group(self):
        streams = [st for st in self._grp if st]
        self._grp = None
        self._cur = None
        n = len(streams)
        pw = [dict() for _ in range(n)]
        pa = [dict() for _ in range(n)]
        for si, st in enumerate(streams):
            for (eng, fn, reads, writes, is_dma, stream) in st:
                for k in reads:
                    pa[si][k] = pa[si].get(k, 0) + 1
                for k in writes:
                    pa[si][k] = pa[si].get(k, 0) + 1
                    pw[si][k] = pw[si].get(k, 0) + 1
        idx = [0] * n
        left = sum(len(st) for st in streams)
        while left:
            for si in range(n):
                if idx[si] >= len(streams[si]):
                    continue
                eng, fn, reads, writes, is_dma, stream = streams[si][idx[si]]
                ok = True
                for k in reads:
                    for s2 in range(si):
                        if pw[s2].get(k, 0) > 0:
                            ok = False
                            break
                    if not ok:
                        break
                if ok:
                    for k in writes:
                        for s2 in range(si):
                            if pa[s2].get(k, 0) > 0:
                                ok = False
                                break
                        if not ok:
                            break
                if not ok:
                    continue
                self._add(eng, fn, reads, writes, is_dma, stream)
                for k in reads:
                    pa[si][k] -= 1
                for k in writes:
                    pa[si][k] -= 1
                    pw[si][k] -= 1
                idx[si] += 1
                left -= 1

    def _add(self, eng, fn, reads, writes, is_dma=False, stream=None):
        if self._cur is not None:
            self._cur.append((eng, fn, tuple(reads), tuple(writes), is_dma, stream))
            return None
        op = _Op(eng, fn, is_dma, stream)
        op.idx = len(self.ops)
        deps = {}
        for k in reads:
            w = self.lastw.get(k)
            if w is not None:
                deps[w] = deps.get(w, "") + "r"
        for k in writes:
            w = self.lastw.get(k)
            if w is not None:
                deps[w] = deps.get(w, "") + "w"
            for r in self.readers.get(k, ()):
                deps[r] = deps.get(r, "") + "a"
        for a_idx, kinds in deps.items():
            a = self.ops[a_idx]
            need = True
            if not a.is_dma and not is_dma and a.eng == eng:
                need = ("r" in kinds) and eng != "pe"
            if need:
                op.waits.append(a_idx)
                a.signal = True
        for k in reads:
            self.readers.setdefault(k, []).append(op.idx)
        for k in writes:
            self.lastw[k] = op.idx
            self.readers[k] = []
        self.ops.append(op)
        return op

    def op(self, eng, fn, reads=(), writes=()):
        return self._add(eng, fn, reads, writes)

    def dma(self, queue, out, in_, reads=(), writes=(), stream="ld", **kw):
        def fn(e, out=out, in_=in_, kw=kw):
            return e.dma_start(out=out, in_=in_, **kw)
        return self._add(queue, fn, reads, writes, is_dma=True, stream=stream)

    def _wait(self, e, s, v):
        key = id(s)
        if self.waited[e].get(key, -1) >= v:
            return
        self.engs[e].wait_ge(s, v)
        self.n_inst[e] += 1
        self.waited[e][key] = v

    def flush(self, barrier=True):
        nc = self.nc
        pend = range(self.emitted, len(self.ops))
        live = set(self.lastw.values())
        for rs in self.readers.values():
            live.update(rs)
        last_on = {}
        for i in pend:
            op = self.ops[i]
            if i in live:
                op.signal = True
            if not op.is_dma:
                last_on[op.eng] = i
        for i in last_on.values():
            self.ops[i].signal = True
        for i in pend:
            op = self.ops[i]
            e = op.eng
            for a_idx in op.waits:
                a = self.ops[a_idx]
                self._wait(e, a.sig_sem, a.sig_val)
            if op.is_dma:
                st = op.stream
                if st not in self.st_sems:
                    self.st_sems[st] = [self.stack.enter_context(nc.semaphore("d_%s_%d" % (st, j)))
                                        for j in range(self.K)]
                    self.st_cnt[st] = 0
                n = self.st_cnt[st]
                self.st_cnt[st] += 1
                s = self.st_sems[st][n % self.K]
                if n >= self.K:
                    self._wait(e, s, 16 * (n // self.K))
                ins = op.fn(self.engs[e])
                ins.then_inc(s, 16)
                op.sig_sem = s
                op.sig_val = 16 * (n // self.K + 1)
            else:
                ins = op.fn(self.engs[e])
                if op.signal:
                    self.cnt[e] += 1
                    ins.then_inc(self.sem[e], 1)
                    op.sig_sem = self.sem[e]
                    op.sig_val = self.cnt[e]
            self.n_inst[e] += 1
            op.fn = None
        self.emitted = len(self.ops)
        if barrier:
            for e in self.engs:
                for st, sems in self.st_sems.items():
                    n = self.st_cnt[st]
                    for j, s in enumerate(sems):
                        nj = (n - j + self.K - 1) // self.K if n > j else 0
                        if nj > 0:
                            self._wait(e, s, 16 * nj)
                for c in COMPUTE:
                    if c != e and self.cnt[c] > 0:
                        self._wait(e, self.sem[c], self.cnt[c])


class Pool:
    def __init__(self, K, name, n, shape, dt, psum=False):
        self.name = name
        self.n = n
        self.i = 0
        alloc = K.ps if psum else K.sb
        self.t = [alloc("%s%d" % (name, j), shape, dt) for j in range(n)]

    def next(self):
        j = self.i % self.n
        self.i += 1
        return self.t[j], (self.name, j)


class KB:
    def __init__(self):
        self.nc = bass.Bass("TRN2", target_bir_lowering=False)
        self.root = ExitStack()
        self.stacks = [self.root]
        self.P = Prog(self.nc, self.root)
        self._uid = 0

    def sb(self, name, shape, dt):
        self._uid += 1
        return self.stacks[-1].enter_context(self.nc.sbuf_tensor("%s_%d" % (name, self._uid), shape, dt))

    def ps(self, name, shape, dt):
        self._uid += 1
        return self.stacks[-1].enter_context(self.nc.psum_tensor("%s_%d" % (name, self._uid), shape, dt))

    def dram(self, name, shape, dt, kind="Internal"):
        return self.nc.dram_tensor(name, shape, dt, kind=kind).ap()

    class _Scope:
        def __init__(self, K):
            self.K = K

        def __enter__(self):
            st = ExitStack()
            self.K.stacks.append(st)
            return st

        def __exit__(self, *a):
            if a[0] is None:
                self.K.P.flush(barrier=True)
            st = self.K.stacks.pop()
            st.close()
            return False

    def scope(self):
        return KB._Scope(self)


def make_consts(K):
    P = K.P
    c = {}
    c["ident_f"] = K.sb("ident_f", [128, 128], F32)
    c["ident_b"] = K.sb("ident_b", [128, 128], BF16)
    idf, idb = c["ident_f"], c["ident_b"]
    P.op("pool", lambda e: e.memset(idf[:], 0.0), writes=["ident_f"])
    P.op("pool", lambda e: e.affine_select(idf[:], idf[:], [[-1, 128]], ALU.not_equal, 1.0,
                                           base=0, channel_multiplier=1),
         reads=["ident_f"], writes=["ident_f"])
    P.op("dve", lambda e: e.tensor_copy(idb[:], idf[:]), reads=["ident_f"], writes=["ident_b"])
    return c


def load_bcast_row(K, name, dram_row, n, queue="sp"):
    t = K.sb(name, [128, n], F32)
    K.P.dma(queue, t[:], dram_row.partition_broadcast(128), writes=[name], stream="cst")
    return t


def load_w_bf16(K, name, w_dram, kchunks, cols, c0=0, key=None):
    t = K.sb(name, [128, kchunks, cols], BF16)
    wr = w_dram.rearrange("(k p) c -> p k c", p=128)
    for k in range(kchunks):
        K.P.dma("pool", t[:, k, :], wr[:, k, c0:c0 + cols], writes=[(key or name, k)], stream="wc")
    return t


def ln_tile(K, c, zt, zk, g_bc, b_bc, gk, bk_, out_t, out_k):
    P = K.P
    st, stk = c["ln_stats"].next()
    mv, mvk = c["ln_mv"].next()
    for i in range(2):
        P.op("dve", lambda e, i=i: e.bn_stats(st[:, i, :], zt[:, i * 512:(i + 1) * 512]),
             reads=[zk], writes=[stk])
    P.op("dve", lambda e: e.bn_aggr(mv[:, 0:2], st[:].rearrange("p a b -> p (a b)")), reads=[stk], writes=[mvk])
    P.op("act", lambda e: e.activation(mv[:, 2:3], mv[:, 1:2], AF.Sqrt, bias=c["eps_ln"][:], scale=1.0),
         reads=[mvk, "eps_ln"], writes=[mvk])
    P.op("dve", lambda e: e.reciprocal(mv[:, 3:4], mv[:, 2:3]), reads=[mvk], writes=[mvk])
    P.op("dve", lambda e: e.scalar_tensor_tensor(mv[:, 4:5], mv[:, 0:1], -1.0, mv[:, 3:4], ALU.mult, ALU.mult),
         reads=[mvk], writes=[mvk])
    P.op("act", lambda e: e.activation(out_t, zt, AF.Identity, bias=mv[:, 4:5], scale=mv[:, 3:4]),
         reads=[zk, mvk], writes=[out_k])
    P.op("dve", lambda e: e.tensor_tensor(out_t, out_t, g_bc, ALU.mult), reads=[out_k, gk], writes=[out_k])
    P.op("pool", lambda e: e.tensor_tensor(out_t, out_t, b_bc, ALU.add), reads=[out_k, bk_], writes=[out_k])


def transpose_to_hT(K, c, src_bf, src_k, dst, dst_k, ncol_chunks=8, dst_off=0):
    P = K.P
    for half in range(0, ncol_chunks, 8):
        n = min(8, ncol_chunks - half)
        pt, ptk = c["tp_b"].next()
        for k in range(n):
            P.op("pe", lambda e, k=k, pt=pt, half=half: e.transpose(pt[:, k, :], src_bf[:, (half + k) * 128:(half + k + 1) * 128],
                                                  c["ident_b"][:]),
                 reads=[src_k, "ident_b"], writes=[ptk])
        eng = c["tp_eng"][c["tp_i"] % 2]
        c["tp_i"] += 1
        if eng == "act":
            P.op("act", lambda e, pt=pt, half=half, n=n: e.copy(dst[:, half:half + n, dst_off:dst_off + 128], pt[:, 0:n, :]),
                 reads=[ptk], writes=[dst_k])
        else:
            P.op("dve", lambda e, pt=pt, half=half, n=n: e.tensor_copy(dst[:, half:half + n, dst_off:dst_off + 128], pt[:, 0:n, :]),
                 reads=[ptk], writes=[dst_k])


def phase_qkv(K, c, x_d, w_in, b_f, qaT, kaT, vaug, ctok, cref):
    P = K.P
    with K.scope():
        wqk = load_w_bf16(K, "wqk", w_in, 8, 2048, 0)
        wv = load_w_bf16(K, "wv", w_in, 8, 1024, 2048)
        wf = load_w_bf16(K, "wf", w_in, 8, 16, 3072)
        negb = K.sb("negb", [16, 1], F32)
        P.dma("sp", negb[:], b_f.rearrange("(h o) -> h o", o=1), writes=["negb"], stream="cst")
        P.op("dve", lambda e: e.tensor_scalar(negb[:], negb[:], -1.0, None, ALU.mult), reads=["negb"], writes=["negb"])
        nl = K.sb("nl", [16, S], F32)
        cn = K.sb("cn", [16, S], F32)
        ones16 = K.sb("ones16", [16, 512], F32)
        P.op("pool", lambda e: e.memset(ones16[:], 1.0), writes=["ones16"])
        xin = Pool(K, "xin", 4, [128, D], F32)
        xT = Pool(K, "xT", 2, [128, 8, 512], BF16)
        tpf = Pool(K, "tpf", 2, [128, 4, 128], F32, psum=True)
        pqk = Pool(K, "pqk", 3, [128, 512], F32, psum=True)
        pv = Pool(K, "pv", 2, [128, 512], F32, psum=True)
        pfl = K.ps("pfl", [16, 512], F32)
        qst = Pool(K, "qst", 4, [128, 512], BF16)
        vst = Pool(K, "vst", 2, [128, 16, 128], BF16)
        for j in range(2):
            P.op("pool", lambda e, j=j: e.memset(vst.t[j][:], 1.0), writes=[("vst", j)])
        e1 = K.sb("e1", [16, 512], F32)
        ev = 0
        for ch in range(8):
            xTc, xTk = xT.next()
            for t in range(4):
                tok0 = ch * 512 + t * 128
                xt, xk = xin.next()
                P.dma("sp", xt[:], x_d[tok0:tok0 + 128, :], writes=[xk], stream="ld")
                for half in range(2):
                    pt, ptk = tpf.next()
                    for k in range(4):
                        kk = half * 4 + k
                        P.op("pe", lambda e, k=k, kk=kk, pt=pt, xt=xt: e.transpose(
                            pt[:, k, :], xt[:, kk * 128:(kk + 1) * 128], c["ident_f"][:]),
                            reads=[xk, "ident_f"], writes=[ptk])
                    if half == 0:
                        P.op("act", lambda e, pt=pt, xTc=xTc, t=t: e.copy(xTc[:, 0:4, t * 128:(t + 1) * 128], pt[:]),
                             reads=[ptk], writes=[xTk])
                    else:
                        P.op("dve", lambda e, pt=pt, xTc=xTc, t=t: e.tensor_copy(xTc[:, 4:8, t * 128:(t + 1) * 128], pt[:]),
                             reads=[ptk], writes=[xTk])
            for cc in range(16):
                pq, pqk_k = pqk.next()
                for k in range(8):
                    P.op("pe", lambda e, k=k, cc=cc, pq=pq, xTc=xTc: e.matmul(
                        pq[:], wqk[:, k, cc * 128:(cc + 1) * 128], xTc[:, k, :], start=(k == 0), stop=(k == 7)),
                        reads=[("wqk", k), xTk], writes=[pqk_k])
                qs, qsk = qst.next()
                if ev % 2 == 0:
                    P.op("act", lambda e, qs=qs, pq=pq: e.copy(qs[:], pq[:]), reads=[pqk_k], writes=[qsk])
                else:
                    P.op("dve", lambda e, qs=qs, pq=pq: e.tensor_copy(qs[:], pq[:]), reads=[pqk_k], writes=[qsk])
                ev += 1
                dst = qaT if cc < 8 else kaT
                hp = (cc % 8) * 2
                for hh in range(2):
                    P.dma("sp", dst[hp + hh, 0:64, ch * 512:(ch + 1) * 512], qs[hh * 64:(hh + 1) * 64, :],
                          reads=[qsk], writes=[("qk_d", cc < 8, hp + hh)], stream="st")
            for t in range(4):
                tok0 = ch * 512 + t * 128
                vs, vsk = vst.next()
                for half in range(2):
                    pvt, pvk = pv.next()
                    for k in range(8):
                        P.op("pe", lambda e, k=k, half=half, pvt=pvt, xTc=xTc, t=t: e.matmul(
                            pvt[:], xTc[:, k, t * 128:(t + 1) * 128], wv[:, k, half * 512:(half + 1) * 512],
                            start=(k == 0), stop=(k == 7)),
                            reads=[("wv", k), xTk], writes=[pvk])
                    src = pvt[:].rearrange("p (h d) -> p h d", d=64)
                    if half == 0:
                        P.op("act", lambda e, vs=vs, src=src: e.copy(vs[:, 0:8, 0:64], src), reads=[pvk], writes=[vsk])
                    else:
                        P.op("dve", lambda e, vs=vs, src=src: e.tensor_copy(vs[:, 8:16, 0:64], src), reads=[pvk], writes=[vsk])
                P.dma("sp", vaug[tok0:tok0 + 128, :, :], vs[:], reads=[vsk], writes=[("vaug_d", tok0 // 128)], stream="st")
            for k in range(8):
                P.op("pe", lambda e, k=k, xTc=xTc: e.matmul(pfl[:], wf[:, k, :], xTc[:, k, :], start=(k == 0), stop=(k == 7)),
                     reads=[("wf", k), xTk], writes=["pfl"])
            P.op("act", lambda e: e.activation(e1[:], pfl[:], AF.Exp, bias=negb[:], scale=-1.0),
                 reads=["pfl", "negb"], writes=["e1"])
            P.op("act", lambda e, ch=ch: e.activation(nl[:, ch * 512:(ch + 1) * 512], e1[:], AF.Ln, bias=1.0, scale=1.0),
                 reads=["e1"], writes=[("nl", ch)])
            init = 0.0 if ch == 0 else cn[:, ch * 512 - 1:ch * 512]
            P.op("dve", lambda e, ch=ch, init=init: e.tensor_tensor_scan(
                cn[:, ch * 512:(ch + 1) * 512], ones16[:], nl[:, ch * 512:(ch + 1) * 512], init, ALU.mult, ALU.add),
                reads=[("nl", ch), "ones16", ("cn", ch - 1)], writes=[("cn", ch)])
        dq = K.sb("dq", [16, S], F32)
        hi = K.sb("hi", [16, S], BF16)
        lo = K.sb("lo", [16, S], BF16)
        onesb = K.sb("onesb", [16, S], BF16)
        P.op("pool", lambda e: e.memset(onesb[:], 1.0), writes=["onesb"])
        allcn = [("cn", ch) for ch in range(8)]
        for ch in range(8):
            sl = slice(ch * 512, (ch + 1) * 512)
            P.op("dve", lambda e, ch=ch, sl=sl: e.tensor_scalar(dq[:, sl], cn[:, sl], cn[:, ch * 512:ch * 512 + 1], -8.0,
                                                              ALU.subtract, ALU.mult),
                 reads=[("cn", ch)], writes=[("dq", ch)])
            P.op("dve", lambda e, sl=sl: e.tensor_copy(hi[:, sl], dq[:, sl]), reads=[("dq", ch)], writes=[("hi", ch)])
            P.op("dve", lambda e, sl=sl: e.tensor_tensor(lo[:, sl], dq[:, sl], hi[:, sl], ALU.subtract),
                 reads=[("dq", ch), ("hi", ch)], writes=[("lo", ch)])
        P.dma("sp", qaT[:, 64, :], hi[:], reads=[("hi", ch) for ch in range(8)], writes=["qa_hi"], stream="st")
        P.dma("sp", qaT[:, 65, :], lo[:], reads=[("lo", ch) for ch in range(8)], writes=["qa_lo"], stream="st")
        P.dma("sp", kaT[:, 64, :], onesb[:], reads=["onesb"], writes=["ka_1"], stream="st")
        P.dma("sp", kaT[:, 65, :], onesb[:], reads=["onesb"], writes=["ka_2"], stream="st")
        pct = pqk.t[0][:].rearrange("p (a b) -> p a b", b=16)
        pctk = ("pqk", 0)
        for j in range(32):
            P.op("pe", lambda e, j=j: e.transpose(pct[:, j, :], cn[:, j * 128:(j + 1) * 128], c["ident_f"][0:16, 0:16]),
                 reads=allcn + ["ident_f"], writes=[pctk])
        P.op("dve", lambda e: e.tensor_copy(ctok[:], pct), reads=[pctk], writes=["ctok"])
        sel0 = K.sb("sel0", [128, 128], F32)
        P.op("pool", lambda e: e.memset(sel0[:], 0.0), writes=["sel0"])
        P.op("pool", lambda e: e.memset(sel0[0:1, :], 1.0), writes=["sel0"])
        pcr = pqk.t[1][:, 0:128]
        pcrk = ("pqk", 1)
        crsrc = K.sb("crsrc", [128, 8, 16], F32)
        P.op("dve", lambda e: e.tensor_copy(crsrc[:], ctok[:, 0:32:4, :]), reads=["ctok"], writes=["crsrc"])
        P.op("pe", lambda e: e.matmul(pcr, sel0[:], crsrc[:].rearrange("p a b -> p (a b)"), start=True, stop=True),
             reads=["sel0", "crsrc"], writes=[pcrk])
        P.op("dve", lambda e: e.tensor_copy(cref[:].rearrange("p a b -> p (a b)"), pcr), reads=[pcrk], writes=["cref"])


def phase_attn(K, c, qaT, kaT, vaug, ctok, cref, attnT):
    P = K.P
    LOOK = 3
    with K.scope():
        qa = Pool(K, "qa", 2, [66, S], BF16)
        ka = Pool(K, "ka", 2, [66, S], BF16)
        va = Pool(K, "va", 2, [128, 32, 128], BF16)
        pS = Pool(K, "pS", 4, [128, 512], F32, psum=True)
        pO = Pool(K, "pO", 2, [128, 512], F32, psum=True)
        pT = Pool(K, "pT", 4, [128, 512], BF16)
        bias = Pool(K, "bias", 3, [128, 32], F32)
        rden = Pool(K, "rden", 2, [128, 512], F32)
        ost = Pool(K, "ost", 3, [64, 512], BF16)
        vview = vaug.rearrange("(j p) h d -> p j h d", p=128)
        heads = {}
        blocks = {}
        sbuf_s = {}

        def load_head(h):
            if h >= 16:
                return
            qt, qk = qa.next()
            kt, kk = ka.next()
            vt, vk = va.next()
            P.dma("sp", qt[:], qaT[h], reads=[("qk_d", True, h), "qa_hi", "qa_lo"], writes=[qk], stream="ld")
            P.dma("sp", kt[:], kaT[h], reads=[("qk_d", False, h), "ka_1", "ka_2"], writes=[kk], stream="ld")
            for jj in range(0, 32, 8):
                P.dma("sp", vt[:, jj:jj + 8, :], vview[:, jj:jj + 8, h, :],
                      reads=[("vaug_d", j) for j in range(jj, jj + 8)], writes=[(vk, jj)], stream="ld")
            heads[h] = (qt, qk, kt, kk, vt, vk)

        def geom(ch, j):
            i = j - 4 * ch
            q0 = 0 if i < 0 else 128 * i
            return i, q0, 512 - q0

        def emit_S(it):
            h, ch, j, nj = it
            qt, qk, kt, kk, vt, vk = heads[h]
            if j == 0:
                bt, bk = bias.next()
                P.op("dve", lambda e, bt=bt, nj=nj, h=h, ch=ch: e.tensor_scalar(
                    bt[:, 0:nj], ctok[:, 0:nj, h], cref[:, ch, h:h + 1], None, ALU.subtract),
                    reads=["ctok", "cref"], writes=[bk])
                po, pok = pO.next()
                blocks[(h, ch)] = (bt, bk, po, pok)
            i, q0, n = geom(ch, j)
            ps, psk = pS.next()
            P.op("pe", lambda e, ps=ps, kt=kt, qt=qt, j=j, q0=q0, n=n, ch=ch: e.matmul(
                ps[:, 0:n], kt[:, j * 128:(j + 1) * 128], qt[:, ch * 512 + q0:ch * 512 + 512],
                start=True, stop=True),
                reads=[qk, kk], writes=[psk])
            sbuf_s[it] = (ps, psk)

        def emit_rest(it):
            h, ch, j, nj = it
            qt, qk, kt, kk, vt, vk = heads[h]
            bt, bk, po, pok = blocks[(h, ch)]
            ps, psk = sbuf_s.pop(it)
            i, q0, n = geom(ch, j)
            pt, ptk = pT.next()
            P.op("act", lambda e, pt=pt, ps=ps, bt=bt, j=j, n=n: e.activation(
                pt[:, 0:n], ps[:, 0:n], AF.Exp, bias=bt[:, j:j + 1], scale=0.125),
                reads=[bk], writes=[ptk, psk])
            if i >= 0:
                P.op("pool", lambda e, pt=pt: e.affine_select(
                    pt[:, 0:128], pt[:, 0:128], [[1, 128]], ALU.is_ge, 0.0, base=0, channel_multiplier=-1),
                    reads=[ptk], writes=[ptk])
            P.op("pe", lambda e, po=po, vt=vt, pt=pt, j=j, q0=q0, n=n, nj=nj: e.matmul(
                po[:, q0:512], vt[:, j, :], pt[:, 0:n], start=(j == 0), stop=(j == nj - 1)),
                reads=[(vk, (j // 8) * 8), ptk], writes=[pok])
            if j == nj - 1:
                rd, rdk = rden.next()
                P.op("dve", lambda e, rd=rd, po=po: e.reciprocal(rd[64:128, :], po[64:128, :]), writes=[rdk, pok])
                os_, osk = ost.next()
                P.op("dve", lambda e, os_=os_, po=po, rd=rd: e.tensor_tensor(os_[:], po[0:64, :], rd[64:128, :], ALU.mult),
                     reads=[rdk], writes=[osk, pok])
                P.dma("sp", attnT[h * 64:(h + 1) * 64, ch * 512:(ch + 1) * 512], os_[:], reads=[osk],
                      writes=[("attn_d", ch)], stream="st")
                del blocks[(h, ch)]
                if ch == 7:
                    load_head(h + 2)

        items = [(h, ch, j, 4 * ch + 4) for h in range(16) for ch in range(8) for j in range(4 * ch + 4)]
        load_head(0)
        load_head(1)
        for k in range(LOOK):
            emit_S(items[k])
        for idx, it in enumerate(items):
            if idx + LOOK < len(items):
                emit_S(items[idx + LOOK])
            emit_rest(it)


def phase_proj_ln(K, c, inT, in_keys, kch, w_o, resid, resid_keys, g_row, b_row, h1, h1T, tag):
    P = K.P
    with K.scope():
        wo = load_w_bf16(K, "wo", w_o, kch, D)
        g_bc = load_bcast_row(K, "g_bc", g_row, D)
        b_bc = load_bcast_row(K, "b_bc", b_row, D)
        c["ln_stats"] = Pool(K, "lnst", 4, [128, 2, 6], F32)
        c["ln_mv"] = Pool(K, "lnmv", 4, [128, 8], F32)
        c["tp_b"] = Pool(K, "tpb", 2, [128, 8, 128], BF16, psum=True)
        c["tp_i"] = 0
        c["tp_eng"] = ("act", "dve")
        ain = Pool(K, "ain", 2, [128, kch, 512], BF16)
        xin = Pool(K, "rin", 4, [128, D], F32)
        pm = Pool(K, "pm", 3, [128, 2, 512], F32, psum=True)
        zt = Pool(K, "zt", 4, [128, D], F32)
        ht = Pool(K, "ht", 4, [128, D], F32)
        hb = Pool(K, "hb", 4, [128, D], BF16)
        hTs = Pool(K, "hTs", 2, [128, 8, 512], BF16)
        inv = inT.rearrange("(k p) t -> p k t", p=128)
        h1Tv = h1T.rearrange("(k p) t -> p k t", p=128)
        abuf = {}
        zs = {}
        stg = {}

        def mm(ti):
            ch, t = ti // 4, ti % 4
            tok0 = ti * 128
            if t == 0:
                a, ak = ain.next()
                for k0 in range(0, kch, 8):
                    P.dma("sp", a[:, k0:k0 + 8, :], inv[:, k0:k0 + 8, ch * 512:(ch + 1) * 512],
                          reads=in_keys(ch), writes=[(ak, k0)], stream="ld")
                abuf[ch] = (a, ak)
            a, ak = abuf[ch]
            xr, xrk = xin.next()
            P.dma("sp", xr[:], resid[tok0:tok0 + 128, :], reads=resid_keys(ti), writes=[xrk], stream="ld")
            pmt, pmk = pm.next()
            for half in range(2):
                for k in range(kch):
                    P.op("pe", lambda e, k=k, half=half: e.matmul(
                        pmt[:, half, :], a[:, k, t * 128:(t + 1) * 128], wo[:, k, half * 512:(half + 1) * 512],
                        start=(k == 0), stop=(k == kch - 1)),
                        reads=[(ak, (k // 8) * 8), ("wo", k)], writes=[(pmk, half)])
            z, zk = zt.next()
            P.op("dve", lambda e: e.scalar_tensor_tensor(
                z[:], xr[:], ALPHA, pmt[:].rearrange("p a b -> p (a b)"), ALU.mult, ALU.add),
                reads=[xrk], writes=[zk, (pmk, 0), (pmk, 1)])
            zs[ti] = (z, zk)

        def ln(ti):
            ch, t = ti // 4, ti % 4
            tok0 = ti * 128
            if t == 0:
                stg[ch] = hTs.next()
            hs, hsk = stg[ch]
            z, zk = zs.pop(ti)
            ho, hok = ht.next()
            ln_tile(K, c, z[:], zk, g_bc[:], b_bc[:], "g_bc", "b_bc", ho[:], hok)
            P.dma("pool", h1[tok0:tok0 + 128, :], ho[:], reads=[hok], writes=[(tag + "h1_d", ti)], stream="st")
            hbt, hbk = hb.next()
            P.op("act", lambda e: e.copy(hbt[:], ho[:]), reads=[hok], writes=[hbk])
            transpose_to_hT(K, c, hbt, hbk, hs, (hsk, t), 8, t * 128)

        for g0 in range(0, 32, 4):
            P.begin_group()
            for ti in range(g0, g0 + 4):
                P.next_stream()
                mm(ti)
                ln(ti)
            P.end_group()
            ch = g0 // 4
            hs, hsk = stg.pop(ch)
            P.dma("pool", h1Tv[:, :, ch * 512:(ch + 1) * 512], hs[:], reads=[(hsk, t) for t in range(4)],
                  writes=[(tag + "h1T_d", ch)], stream="st")


def epilogue_s1(K, c, z, zk, tok0, g_bc, b_bc, p_d):
    P = K.P
    h2, h2k = c["h2"].next()
    ln_tile(K, c, z[:], zk, g_bc[:], b_bc[:], "g2_bc", "b2_bc", h2[:], h2k)
    hbt, hbk = c["hb2"].next()
    P.op("act", lambda e: e.copy(hbt[:], h2[:]), reads=[h2k], writes=[hbk])
    h2T, h2Tk = c["h2T"].next()
    transpose_to_hT(K, c, hbt, hbk, h2T, h2Tk, 8, 0)
    pt_, ptk = c["pin"].next()
    P.dma("sp", pt_[:], p_d[tok0:tok0 + 128, :], writes=[ptk], stream="ld")
    pb, pbk = c["pb"].next()
    P.op("dve", lambda e: e.tensor_copy(pb[:], pt_[:]), reads=[ptk], writes=[pbk])
    pT, pTk = c["pT"].next()
    transpose_to_hT(K, c, pb, pbk, pT, pTk, 2, 0)
    return (h2, h2k, h2T, h2Tk, pT, pTk, tok0)


def epilogue_s2(K, c, ctx, wpg, wpp, out_d, out_key, outT_stage):
    P = K.P
    h2, h2k, h2T, h2Tk, pT, pTk, tok0 = ctx
    pgp, pgpk = c["pg"].next()
    sg, sgk = c["sg"].next()
    for half in range(2):
        cs = slice(half * 512, (half + 1) * 512)
        for k in range(8):
            P.op("pe", lambda e, k=k, cs=cs: e.matmul(pgp[:, 0, :], h2T[:, k, :], wpg[:, k, cs], start=(k == 0), stop=(k == 7)),
                 reads=[h2Tk, ("wpg", k)], writes=[(pgpk, 0)])
        for k in range(2):
            P.op("pe", lambda e, k=k, cs=cs: e.matmul(pgp[:, 1, :], pT[:, k, :], wpp[:, k, cs], start=(k == 0), stop=(k == 1)),
                 reads=[pTk, ("wpp", k)], writes=[(pgpk, 1)])
        P.op("act", lambda e, cs=cs: e.activation(sg[:, cs], pgp[:, 0, :], AF.Sigmoid), writes=[(sgk, half), (pgpk, 0)])
        P.op("dve", lambda e, cs=cs: e.tensor_tensor(sg[:, cs], pgp[:, 1, :], sg[:, cs], ALU.mult),
             reads=[(sgk, half)], writes=[(sgk, half), (pgpk, 1)])
    h3, h3k = c["h3"].next()
    P.op("pool", lambda e: e.tensor_tensor(h3[:], h2[:], sg[:], ALU.add), reads=[h2k, (sgk, 0), (sgk, 1)], writes=[h3k])
    P.dma("pool", out_d[tok0:tok0 + 128, :], h3[:], reads=[h3k], writes=[(out_key, tok0 // 128)], stream="st")
    if outT_stage is not None:
        hs, hsk, toff = outT_stage
        hb3, hb3k = c["hb2"].next()
        P.op("act", lambda e: e.copy(hb3[:], h3[:]), reads=[h3k], writes=[hb3k])
        transpose_to_hT(K, c, hb3, hb3k, hs, hsk, 8, toff)


def epilogue_setup(K, c, g_row, b_row, w_pg, w_pp, npg=1, nb=2):
    g_bc = load_bcast_row(K, "g2_bc", g_row, D)
    b_bc = load_bcast_row(K, "b2_bc", b_row, D)
    wpg = load_w_bf16(K, "wpg", w_pg, 8, D)
    wpp = load_w_bf16(K, "wpp", w_pp, 2, D)
    c["ln_stats"] = Pool(K, "lnst", nb, [128, 2, 6], F32)
    c["ln_mv"] = Pool(K, "lnmv", nb, [128, 8], F32)
    c["tp_b"] = Pool(K, "tpb", 2, [128, 8, 128], BF16, psum=True)
    c["tp_i"] = 0
    c["tp_eng"] = ("act", "dve")
    c["h2"] = Pool(K, "h2", nb, [128, D], F32)
    c["hb2"] = Pool(K, "hb2", nb, [128, D], BF16)
    c["h2T"] = Pool(K, "h2T", nb, [128, 8, 128], BF16)
    c["pin"] = Pool(K, "pin", nb, [128, 256], F32)
    c["pb"] = Pool(K, "pb", nb, [128, 256], BF16)
    c["pT"] = Pool(K, "pT", nb, [128, 2, 128], BF16)
    c["pg"] = Pool(K, "pg", npg, [128, 2, 512], F32, psum=True)
    c["sg"] = Pool(K, "sg", max(1, nb // 2), [128, D], F32)
    c["h3"] = Pool(K, "h3", nb, [128, D], F32)
    return g_bc, b_bc, wpg, wpp


def phase_ffn(K, c, h1, h1T, w_g, w_u, w_d, g_row, b_row, w_pg, w_pp, p_d, out_d, outT_d, actT_d):
    P = K.P
    NJ = FFN // 128
    h1Tv = h1T.rearrange("(k p) t -> p k t", p=128)
    with K.scope():
        hres = K.sb("hres", [128, 8, S], BF16)
        for ch in range(8):
            P.dma("sp", hres[:, :, ch * 512:(ch + 1) * 512], h1Tv[:, :, ch * 512:(ch + 1) * 512],
                  reads=[("l0h1T_d", ch)], writes=[("hres", ch)], stream="ld")
        wgu = Pool(K, "wgu", 3, [128, 2, 8, 256], BF16)
        pgu = Pool(K, "pgu", 3, [128, 2, 512], F32, psum=True)
        sgt = Pool(K, "sgt", 3, [128, 512], F32)
        ast = Pool(K, "ast", 3, [128, S], BF16)
        wgr = w_g.rearrange("(k p) c -> p k c", p=128)
        wur = w_u.rearrange("(k p) c -> p k c", p=128)
        for j2 in range(NJ // 2):
            wt, wtk = wgu.next()
            P.dma("pool", wt[:, 0], wgr[:, :, j2 * 256:(j2 + 1) * 256], writes=[(wtk, 0)], stream="wc")
            P.dma("pool", wt[:, 1], wur[:, :, j2 * 256:(j2 + 1) * 256], writes=[(wtk, 1)], stream="wc")
            for jj in range(2):
                j = j2 * 2 + jj
                a_, ak = ast.next()
                for blk in range(8):
                    bs = slice(blk * 512, (blk + 1) * 512)
                    pg_, pgk_ = pgu.next()
                    for gu in range(2):
                        for k in range(8):
                            P.op("pe", lambda e, pg_=pg_, wt=wt, gu=gu, k=k, jj=jj, bs=bs: e.matmul(
                                pg_[:, gu, :], wt[:, gu, k, jj * 128:(jj + 1) * 128], hres[:, k, bs],
                                start=(k == 0), stop=(k == 7)),
                                reads=[(wtk, gu), ("hres", blk)], writes=[(pgk_, gu)])
                    sg_, sgk_ = sgt.next()
                    P.op("act", lambda e, sg_=sg_, pg_=pg_: e.activation(sg_[:], pg_[:, 0, :], AF.Silu),
                         writes=[sgk_, (pgk_, 0)])
                    P.op("dve", lambda e, a_=a_, bs=bs, sg_=sg_, pg_=pg_: e.tensor_tensor(a_[:, bs], sg_[:], pg_[:, 1, :], ALU.mult),
                         reads=[sgk_], writes=[(ak, blk), (pgk_, 1)])
                P.dma("sp", actT_d[j * 128:(j + 1) * 128, :], a_[:], reads=[(ak, b) for b in range(8)],
                      writes=[("actT_d", j)], stream="st")
    with K.scope():
        g_bc, b_bc, wpg, wpp = epilogue_setup(K, c, g_row, b_row, w_pg, w_pp, npg=1, nb=3)
        wd = load_w_bf16(K, "wd", w_d, NJ, D)
        ain = Pool(K, "fain", 4, [128, NJ, 128], BF16)
        pf_ = Pool(K, "pf", 2, [128, 2, 512], F32, psum=True)
        h1in = Pool(K, "h1in", 4, [128, D], F32)
        zt = Pool(K, "zt", 4, [128, D], F32)
        hTs = Pool(K, "hTs", 2, [128, 8, 512], BF16)
        actv = actT_d.rearrange("(j p) t -> p j t", p=128)
        outTv = outT_d.rearrange("(k p) t -> p k t", p=128) if outT_d is not None else None

        def down(ti):
            a, ak = ain.next()
            for j0 in range(0, NJ, 11):
                P.dma("sp", a[:, j0:j0 + 11, :], actv[:, j0:j0 + 11, ti * 128:(ti + 1) * 128],
                      reads=[("actT_d", j) for j in range(j0, j0 + 11)], writes=[(ak, j0)], stream="ld")
            h1t, h1k = h1in.next()
            P.dma("sp", h1t[:], h1[ti * 128:(ti + 1) * 128, :], reads=[("l0h1_d", ti)], writes=[h1k], stream="ld")
            pf, pfk = pf_.next()
            for half in range(2):
                for j in range(NJ):
                    P.op("pe", lambda e, j=j, half=half: e.matmul(
                        pf[:, half, :], a[:, j, :], wd[:, j, half * 512:(half + 1) * 512],
                        start=(j == 0), stop=(j == NJ - 1)),
                        reads=[(ak, (j // 11) * 11), ("wd", j)], writes=[(pfk, half)])
            z, zk = zt.next()
            P.op("dve", lambda e: e.scalar_tensor_tensor(
                z[:], h1t[:], ALPHA, pf[:].rearrange("p a b -> p (a b)"), ALU.mult, ALU.add),
                reads=[h1k], writes=[zk, (pfk, 0), (pfk, 1)])
            return z, zk

        for blk in range(8):
            hs, hsk = hTs.next() if outTv is not None else (None, None)
            P.begin_group()
            for t in range(4):
                ti = blk * 4 + t
                P.next_stream()
                z, zk = down(ti)
                ctx = epilogue_s1(K, c, z, zk, ti * 128, g_bc, b_bc, p_d)
                epilogue_s2(K, c, ctx, wpg, wpp, out_d, "out_d", (hs, (hsk, t), t * 128) if outTv is not None else None)
            P.end_group()
            if outTv is not None:
                P.dma("pool", outTv[:, :, blk * 512:(blk + 1) * 512], hs[:], reads=[(hsk, t) for t in range(4)],
                      writes=[("outT_d", blk)], stream="st")


def phase_ssd_proj(K, c, hT_d, hT_key, w_in, conv_w, conv_b, dt_bias, xs_d, Btok_d, BT_d, CT_d, dt_tok):
    P = K.P
    with K.scope():
        wx = load_w_bf16(K, "wx", w_in, 8, 4096, 2048)
        wdt = load_w_bf16(K, "wdt", w_in, 8, 32, 6144)
        dtb = load_bcast_row(K, "dtb", dt_bias, 32)
        pxb = Pool(K, "pxb", 4, [128, 512], F32, psum=True)
        tpf = Pool(K, "tpf", 2, [128, 4, 128], F32, psum=True)
        tpb = Pool(K, "tpb", 1, [128, 8, 128], BF16, psum=True)
        pdt = K.ps("pdt", [128, 512], F32)
        cwb = K.sb("cwb", [128, 32, 5], F32)
        with K.scope():
            cw5 = K.sb("cw5", [5, 4096], F32)
            P.dma("sp", cw5[0:4, :], conv_w, writes=["cw5a"], stream="cst")
            P.dma("sp", cw5[4:5, :], conv_b.rearrange("(o c) -> o c", o=1), writes=["cw5b"], stream="cst")
            pcw = pxb.t[0][:, 0:160].rearrange("p (a b) -> p a b", b=5)
            for i in range(32):
                P.op("pe", lambda e, i=i: e.transpose(pcw[:, i, :], cw5[:, i * 128:(i + 1) * 128], c["ident_f"][0:5, 0:5]),
                     reads=["cw5a", "cw5b", "ident_f"], writes=[("pxb", 0)])
            P.op("dve", lambda e: e.tensor_copy(cwb[:], pcw), writes=["cwb", ("pxb", 0)])
        carry = K.sb("carry", [128, 32, 3], F32)
        P.op("pool", lambda e: e.memset(carry[:], 0.0), writes=[("carry", i) for i in range(32)])
        hin = Pool(K, "hin", 2, [128, 8, 512], BF16)
        pre = Pool(K, "pre", 8, [128, 515], F32)
        acc = Pool(K, "acc", 8, [128, 512], F32)
        sil = Pool(K, "sil", 12, [128, 512], F32)
        silb = Pool(K, "silb", 12, [128, 512], BF16)
        xst = Pool(K, "xst", 4, [128, 2048], F32)
        bst = Pool(K, "bst", 4, [128, 1024], BF16)
        e1 = Pool(K, "e1", 2, [128, 128], F32)
        hTv = hT_d.rearrange("(k p) t -> p k t", p=128)
        for ch in range(8):
            hi_, hik = hin.next()
            P.dma("sp", hi_[:], hTv[:, :, ch * 512:(ch + 1) * 512], reads=hT_key(ch), writes=[hik], stream="ld")
            for t in range(4):
                for k in range(8):
                    P.op("pe", lambda e, k=k, t=t, hi_=hi_: e.matmul(pdt[:, t * 32:(t + 1) * 32], hi_[:, k, t * 128:(t + 1) * 128],
                                                                    wdt[:, k, :], start=(k == 0), stop=(k == 7)),
                         reads=[hik, ("wdt", k)], writes=["pdt"])
            ee, eek = e1.next()
            P.op("dve", lambda e, ee=ee: e.tensor_tensor(ee[:].rearrange("p (t r) -> p t r", t=4),
                                                         pdt[:, 0:128].rearrange("p (t r) -> p t r", t=4), bc_mid(dtb[:], 4), ALU.add),
                 reads=["dtb"], writes=[eek, "pdt"])
            P.op("act", lambda e, ee=ee: e.activation(ee[:], ee[:], AF.Exp), reads=[eek], writes=[eek])
            P.op("act", lambda e, ee=ee, ch=ch: e.activation(dt_tok[:, ch * 4:(ch + 1) * 4, :].rearrange("p t r -> p (t r)"), ee[:],
                                                             AF.Ln, bias=1.0, scale=1.0),
                 reads=[eek], writes=[("dt_tok", ch * 4 + t) for t in range(4)])
            xs_t = [xst.next() for _ in range(4)]
            bs_t = [bst.next() for _ in range(4)]
            def do_tr(i4, outs):
                if i4 < 4:
                    for t in range(4):
                        pt, ptk = tpf.next()
                        for ii in range(4):
                            so, sok = outs[ii]
                            P.op("pe", lambda e, pt=pt, ii=ii, so=so, t=t: e.transpose(
                                pt[:, ii, :], so[:, t * 128:(t + 1) * 128], c["ident_f"][:]),
                                reads=[sok, "ident_f"], writes=[ptk])
                        xs_, xsk = xs_t[t]
                        if t % 2 == 0:
                            P.op("act", lambda e, xs_=xs_, pt=pt, i4=i4: e.copy(
                                xs_[:, i4 * 512:(i4 + 1) * 512], pt[:].rearrange("p a b -> p (a b)")),
                                reads=[ptk], writes=[(xsk, i4)])
                        else:
                            P.op("dve", lambda e, xs_=xs_, pt=pt, i4=i4: e.tensor_copy(
                                xs_[:, i4 * 512:(i4 + 1) * 512], pt[:].rearrange("p a b -> p (a b)")),
                                reads=[ptk], writes=[(xsk, i4)])
                elif i4 < 6:
                    for t in range(4):
                        pt, ptk = tpb.next()
                        for ii in range(4):
                            so, sok = outs[ii]
                            P.op("pe", lambda e, pt=pt, ii=ii, so=so, t=t: e.transpose(
                                pt[:, ii, :], so[:, t * 128:(t + 1) * 128], c["ident_b"][:]),
                                reads=[sok, "ident_b"], writes=[ptk])
                        bs_, bsk = bs_t[t]
                        gi = i4 - 4
                        P.op("dve", lambda e, bs_=bs_, pt=pt, gi=gi: e.tensor_copy(
                            bs_[:, gi * 512:(gi + 1) * 512], pt[:, 0:4, :].rearrange("p a b -> p (a b)")),
                            reads=[ptk], writes=[(bsk, gi)])

            actx = {}
            bouts = {}

            def stageA(i4):
                lst = []
                for ii in range(4):
                    P.next_stream()
                    i = i4 * 4 + ii
                    px, pxk = pxb.next()
                    for k in range(8):
                        P.op("pe", lambda e, k=k, i=i, px=px, hi_=hi_: e.matmul(
                            px[:], wx[:, k, i * 128:(i + 1) * 128], hi_[:, k, :], start=(k == 0), stop=(k == 7)),
                            reads=[hik, ("wx", k)], writes=[pxk])
                    pr, prk = pre.next()
                    P.op("act", lambda e, pr=pr, px=px: e.copy(pr[:, 3:515], px[:]), writes=[(prk, 1), pxk])
                    P.op("pool", lambda e, pr=pr, i=i: e.tensor_copy(pr[:, 0:3], carry[:, i, :]), reads=[("carry", i)], writes=[(prk, 0)])
                    ac, ack = acc.next()
                    P.op("pool", lambda e, ac=ac, pr=pr, i=i: e.tensor_scalar(
                        ac[:], pr[:, 3:515], cwb[:, i, 3:4], cwb[:, i, 4:5], ALU.mult, ALU.add),
                        reads=[(prk, 1), "cwb"], writes=[ack])
                    lst.append((i, pr, prk, ac, ack))
                actx[i4] = lst

            def stageB(i4):
                outs = []
                for (i, pr, prk, ac, ack) in actx.pop(i4):
                    P.next_stream()
                    for kk in (2, 1, 0):
                        P.op("dve", lambda e, ac=ac, pr=pr, i=i, kk=kk: e.scalar_tensor_tensor(
                            ac[:], pr[:, kk:kk + 512], cwb[:, i, kk:kk + 1], ac[:], ALU.mult, ALU.add),
                            reads=[(prk, 0), (prk, 1), "cwb", ack], writes=[ack])
                    P.op("dve", lambda e, pr=pr, i=i: e.tensor_copy(carry[:, i, :], pr[:, 512:515]),
                         reads=[(prk, 1)], writes=[("carry", i)])
                    if i < 16:
                        so, sok = sil.next()
                    else:
                        so, sok = silb.next()
                    P.op("act", lambda e, so=so, ac=ac: e.activation(so[:], ac[:], AF.Silu), reads=[ack], writes=[sok])
                    outs.append((so, sok))
                    if i >= 16:
                        g = (i - 16) % 8
                        dst = BT_d if i < 24 else CT_d
                        P.dma("act", dst[g, :, ch * 512:(ch + 1) * 512], so[:], reads=[sok],
                              writes=[("BC_d", i < 24, g, ch)], stream="st")
                bouts[i4] = outs

            for step in range(10):
                if step <= 8:
                    P.begin_group()
                    if step < 8:
                        stageA(step)
                    if step >= 1:
                        stageB(step - 1)
                    P.end_group()
                if step >= 2:
                    do_tr(step - 2, bouts.pop(step - 2))
            for t in range(4):
                ti = ch * 4 + t
                xs_, xsk = xs_t[t]
                bs_, bsk = bs_t[t]
                P.dma("pool", xs_d[ti * 128:(ti + 1) * 128, :], xs_[:], reads=[(xsk, q) for q in range(4)],
                      writes=[("xs_d", ti)], stream="st")
                P.dma("pool", Btok_d[ti * 128:(ti + 1) * 128, :], bs_[:], reads=[(bsk, q) for q in range(2)],
                      writes=[("Btok_d", ti)], stream="st")


def bc_mid(ap2d, n):
    sh = list(ap2d.shape)
    return ap2d.unsqueeze(1).broadcast_to([sh[0], n, sh[1]])


def bc_last(ap2d, n):
    sh = list(ap2d.shape)
    return ap2d.unsqueeze(2).broadcast_to([sh[0], sh[1], n])


def phase_ssd_core(K, c, xs_d, Btok_d, BT_d, CT_d, dt_tok, a_log, d_skip, y_d):
    P = K.P
    with K.scope():
        tri = K.sb("tri", [128, 128], F32)
        P.op("pool", lambda e: e.memset(tri[:], 1.0), writes=["tri"])
        P.op("pool", lambda e: e.affine_select(tri[:], tri[:], [[1, 128]], ALU.is_ge, 0.0, base=0, channel_multiplier=-1),
             reads=["tri"], writes=["tri"])
        sel127 = K.sb("sel127", [128, 128], F32)
        P.op("pool", lambda e: e.memset(sel127[:], 1.0), writes=["sel127"])
        P.op("pool", lambda e: e.affine_select(sel127[:], sel127[:], [[0, 128]], ALU.is_equal, 0.0, base=-127, channel_multiplier=1),
             reads=["sel127"], writes=["sel127"])
        ind = K.sb("ind", [96, 32, 128], BF16)
        P.op("pool", lambda e: e.memset(ind[:], 1.0), writes=["ind"])
        for q in range(3):
            P.op("pool", lambda e, q=q: e.affine_select(ind[32 * q:32 * q + 32], ind[32 * q:32 * q + 32], [[-1, 32], [0, 128]],
                                                        ALU.is_equal, 0.0, base=0, channel_multiplier=1),
                 reads=["ind"], writes=["ind"])
        a_bc = load_bcast_row(K, "a_bc", a_log, 32)
        P.op("act", lambda e: e.activation(a_bc[:], a_bc[:], AF.Exp), reads=["a_bc"], writes=["a_bc"])
        P.op("dve", lambda e: e.tensor_scalar(a_bc[:], a_bc[:], -1.0, None, ALU.mult), reads=["a_bc"], writes=["a_bc"])
        dsk = load_bcast_row(K, "dsk", d_skip, 32)
        hT = K.sb("hT", [128, 2048], F32)
        hTb = K.sb("hTb", [128, 2048], BF16)
        P.op("pool", lambda e: e.memset(hT[:], 0.0), writes=["hT"])
        P.op("pool", lambda e: e.memset(hTb[:], 0.0), writes=["hTb"])
        xsc = Pool(K, "xsc", 3, [128, 2048], F32)
        btc = Pool(K, "btc", 3, [128, 1024], BF16)
        BTc = Pool(K, "BTc", 3, [128, 8, 128], BF16)
        CTc = Pool(K, "CTc", 3, [128, 8, 128], BF16)
        sm = Pool(K, "sm", 3, [128, 8, 32], F32)
        dap = Pool(K, "dap", 3, [128, 128], F32)
        for j in range(3):
            P.op("pool", lambda e, j=j: e.memset(dap.t[j][:], 0.0), writes=[("dap", j)])
        dT = Pool(K, "dT", 3, [96, 2, 128], BF16)
        dsc = Pool(K, "dsc", 3, [96, 4, 128], F32)
        dec = Pool(K, "dec", 4, [128, 4, 128], F32)
        MT = Pool(K, "MT", 16, [128, 4, 128], BF16)
        xdt = Pool(K, "xdt", 3, [128, 32, 64], BF16)
        xdte = Pool(K, "xdte", 3, [128, 32, 64], BF16)
        ydg = Pool(K, "ydg", 1, [128, 2048], F32)
        yraw = Pool(K, "yraw", 1, [128, 8, 512], F32)
        stT = K.sb("stT", [128, 2048], F32)
        yo = Pool(K, "yo", 2, [128, 2048], F32)
        pmisc = K.ps("pmisc", [128, 512], F32)
        pcbp = Pool(K, "pcb", 1, [128, 512], F32, psum=True)
        cbs = Pool(K, "cbs", 2, [128, 2, 512], F32)
        pD = Pool(K, "pD", 2, [128, 512], F32, psum=True)
        pY = Pool(K, "pY", 2, [128, 512], F32, psum=True)
        pStp = Pool(K, "pSt", 2, [128, 512], F32, psum=True)
        BTv = BT_d.rearrange("g n t -> n g t")
        CTv = CT_d.rearrange("g n t -> n g t")
        st = {}

        sta = {}

        def pre_a(ch):
            P.next_stream()
            ts = slice(ch * 128, (ch + 1) * 128)
            xs_, xsk = xsc.next()
            bt_, btk = btc.next()
            BT_, BTk = BTc.next()
            CT_, CTk = CTc.next()
            P.dma("sp", xs_[:], xs_d[ts, :], reads=[("xs_d", ch)], writes=[xsk], stream="ld")
            P.dma("sp", bt_[:], Btok_d[ts, :], reads=[("Btok_d", ch)], writes=[btk], stream="ld")
            P.dma("sp", BT_[:], BTv[:, :, ts], reads=[("BC_d", True, g, ch // 4) for g in range(8)], writes=[BTk], stream="ld")
            P.dma("sp", CT_[:], CTv[:, :, ts], reads=[("BC_d", False, g, ch // 4) for g in range(8)], writes=[CTk], stream="ld")
            s_, sk = sm.next()
            dtc = dt_tok[:, ch, :]
            _, dacs, wdte, el, cdb, lastb, tmp = (s_[:, q, :] for q in range(7))
            dp_, dpk = dap.next()
            da = dp_[:, 0:32]
            P.op("dve", lambda e: e.tensor_tensor(dp_[:, 0:96].rearrange("p (q r) -> p q r", q=3), bc_mid(dtc, 3),
                                                  bc_mid(a_bc[:], 3), ALU.mult),
                 reads=[("dt_tok", ch), "a_bc"], writes=[dpk])
            P.op("pe", lambda e: e.matmul(pmisc[:, 0:32], tri[:], da, start=True, stop=True), reads=["tri", dpk], writes=["pmisc"])
            P.op("act", lambda e: e.copy(dacs, pmisc[:, 0:32]), writes=["pmisc", (sk, "dacs")])
            P.op("pe", lambda e: e.matmul(pmisc[:, 32:64], sel127[:], dacs, start=True, stop=True),
                 reads=["sel127", (sk, "dacs")], writes=["pmisc"])
            P.op("pe", lambda e: e.matmul(pmisc[:, 64:192], dp_[:], tri[:], start=True, stop=True), reads=["tri", dpk], writes=["pmisc"])
            d_, dk = dT.next()
            w_, wk = dsc.next()
            src = pmisc[0:96, 64:192]
            hb_, r1, mb_, r2 = (w_[:, q, :] for q in range(4))
            P.op("act", lambda e: e.copy(d_[:, 0, :], src), writes=["pmisc", (dk, "h")])
            P.op("dve", lambda e: e.tensor_tensor(r1, src, d_[:, 0, :], ALU.subtract), reads=[(dk, "h")], writes=["pmisc", (wk, 1)])
            P.op("act", lambda e: e.copy(d_[:, 1, :], r1), reads=[(wk, 1)], writes=[(dk, "m")])
            P.op("dve", lambda e: e.tensor_tensor(r2[64:96], r1[64:96], d_[64:96, 1, :], ALU.subtract),
                 reads=[(wk, 1), (dk, "m")], writes=[(wk, 3)])
            P.op("act", lambda e: e.copy(d_[32:64, 0, :], d_[32:64, 1, :]), reads=[(dk, "m"), (dk, "h")], writes=[(dk, "s1")])
            P.op("dve", lambda e: e.tensor_copy(d_[64:96, 0, :], r2[64:96]), reads=[(wk, 3), (dk, "h")], writes=[(dk, "s2")])
            P.op("dve", lambda e: e.tensor_scalar(d_[:, 1, :], d_[:, 0, :], -1.0, None, ALU.mult),
                 reads=[(dk, "s1"), (dk, "s2"), (dk, "h"), (wk, 3)], writes=[(dk, 1), (dk, "m")])
            P.op("act", lambda e: e.copy(lastb, pmisc[:, 32:64]), writes=["pmisc", (sk, "last")])
            P.op("act", lambda e: e.activation(cdb, pmisc[:, 32:64], AF.Exp), writes=["pmisc", (sk, "cdb")])
            P.op("act", lambda e: e.activation(el, dacs, AF.Exp), reads=[(sk, "dacs")], writes=[(sk, "el")])
            P.op("dve", lambda e: e.tensor_tensor(tmp, lastb, dacs, ALU.subtract), reads=[(sk, "last"), (sk, "dacs")], writes=[(sk, "tmp")])
            P.op("act", lambda e: e.activation(tmp, tmp, AF.Exp), reads=[(sk, "tmp")], writes=[(sk, "tmp")])
            P.op("dve", lambda e: e.tensor_tensor(wdte, tmp, dtc, ALU.mult), reads=[(sk, "tmp"), ("dt_tok", ch)], writes=[(sk, "wdte")])
            xd, xdk = xdt.next()
            xe, xek = xdte.next()
            xs3 = xs_[:].rearrange("p (r d) -> p r d", d=64)
            P.op("dve", lambda e: e.tensor_tensor(xd[:], xs3, bc_last(dtc, 64), ALU.mult), reads=[xsk, ("dt_tok", ch)], writes=[xdk])
            P.op("pool", lambda e: e.tensor_tensor(xe[:], xs3, bc_last(wdte, 64), ALU.mult), reads=[xsk, (sk, "wdte")], writes=[xek])
            sta[ch] = (xs_, xsk, xs3, bt_, btk, BT_, BTk, CT_, CTk, sk, el, cdb, xd, xdk, xe, xek, d_, dk)

        def pre_b(ch):
            xs_, xsk, xs3, bt_, btk, BT_, BTk, CT_, CTk, sk, el, cdb, xd, xdk, xe, xek, d_, dk = sta.pop(ch)
            mts = []
            P.next_stream()
            cb_, cbk = cbs.next()
            pcb, pcbk = pcbp.next()
            for half in range(2):
                for q in range(4):
                    P.op("pe", lambda e, half=half, q=q: e.matmul(pcb[:, q * 128:(q + 1) * 128], BT_[:, half * 4 + q, :],
                                                                  CT_[:, half * 4 + q, :], start=True, stop=True),
                         reads=[BTk, CTk], writes=[pcbk])
                P.op("act", lambda e, half=half: e.copy(cb_[:, half, :], pcb[:]), writes=[pcbk, (cbk, half)])
            for g in range(8):
                P.next_stream()
                pd_, pdk = pD.next()
                P.op("pe", lambda e, pd_=pd_, g=g: e.matmul(
                    pd_[:], d_[:, 1, :], ind[:, 4 * g:4 * g + 4, :].rearrange("p a b -> p (a b)"), start=True, stop=False),
                    reads=["ind", (dk, 1)], writes=[pdk])
                for r in range(4):
                    P.op("pe", lambda e, pd_=pd_, g=g, r=r: e.matmul(
                        pd_[:, r * 128:(r + 1) * 128], ind[:, 4 * g + r, :], d_[:, 0, :], start=False, stop=(r == 3)),
                        reads=["ind", (dk, 1), (dk, "s1"), (dk, "s2"), (dk, "h")], writes=[pdk])
                de, dek = dec.next()
                P.op("act", lambda e, de=de, pd_=pd_: e.activation(de[:].rearrange("p a b -> p (a b)"), pd_[:], AF.Exp),
                     writes=[dek, pdk])
                P.op("pool", lambda e, de=de: e.affine_select(de[:], de[:], [[0, 4], [1, 128]], ALU.is_ge, 0.0, base=0, channel_multiplier=-1),
                     reads=[dek], writes=[dek])
                mt, mtk = MT.next()
                P.op("dve", lambda e, mt=mt, de=de, g=g: e.tensor_tensor(
                    mt[:], de[:], bc_mid(cb_[:, g // 4, (g % 4) * 128:(g % 4) * 128 + 128], 4), ALU.mult),
                     reads=[dek, (cbk, g // 4)], writes=[mtk])
                mts.append((mt, mtk))
            st[ch] = (xs_, xsk, xs3, bt_, btk, CT_, CTk, sk, el, cdb, xd, xdk, xe, xek, mts)

        def main(ch):
            ts = slice(ch * 128, (ch + 1) * 128)
            xs_, xsk, xs3, bt_, btk, CT_, CTk, sk, el, cdb, xd, xdk, xe, xek, mts = st.pop(ch)
            yr, yrk = yraw.next()
            for g in range(8):
                P.next_stream()
                gs = slice(g * 256, (g + 1) * 256)
                mt, mtk = mts[g]
                py, pyk = pY.next()
                for r in range(4):
                    P.op("pe", lambda e, py=py, mt=mt, g=g, r=r: e.matmul(
                        py[:, r * 64:(r + 1) * 64], mt[:, r, :], xd[:, 4 * g + r, :], start=True, stop=True),
                        reads=[mtk, xdk], writes=[pyk])
                P.op("pe", lambda e, py=py, g=g, gs=gs: e.matmul(py[:, 256:512], CT_[:, g, :], hTb[:, gs], start=True, stop=True),
                     reads=[CTk, "hTb"], writes=[pyk])
                ps_, psk_ = pStp.next()
                P.op("pe", lambda e, g=g, ps_=ps_: e.matmul(
                    ps_[:, 0:256], bt_[:, g * 128:(g + 1) * 128], xe[:, 4 * g:4 * g + 4, :].rearrange("p a b -> p (a b)"),
                    start=True, stop=True),
                    reads=[btk, xek], writes=[psk_])
                P.op("act", lambda e, py=py, g=g: e.copy(yr[:, g, :], py[:]), writes=[pyk, (yrk, g)])
                P.op("act", lambda e, gs=gs, ps_=ps_: e.copy(stT[:, gs], ps_[:, 0:256]), writes=[psk_, ("stT", g)])
            P.next_stream()
            P.op("dve", lambda e: e.tensor_tensor(hT[:].rearrange("p (r d) -> p r d", d=64), hT[:].rearrange("p (r d) -> p r d", d=64),
                                                  bc_last(cdb, 64), ALU.mult),
                 reads=["hT", (sk, "cdb")], writes=["hT"])
            P.op("dve", lambda e: e.tensor_tensor(hT[:], hT[:], stT[:], ALU.add),
                 reads=["hT"] + [("stT", g) for g in range(8)], writes=["hT"])
            P.op("act", lambda e: e.copy(hTb[:], hT[:]), reads=["hT"], writes=["hTb"])
            P.next_stream()
            y_, yk = yo.next()
            yd, ydk = ydg.next()
            yrall = [(yrk, g) for g in range(8)]
            P.op("dve", lambda e: e.tensor_tensor(
                y_[:].rearrange("p (g r d) -> p g r d", g=8, r=4), yr[:, :, 256:512].rearrange("p g (r d) -> p g r d", d=64),
                el.rearrange("p (g r) -> p g r", r=4).unsqueeze(3).broadcast_to([128, 8, 4, 64]), ALU.mult),
                reads=yrall + [(sk, "el")], writes=[yk])
            P.op("pool", lambda e: e.tensor_tensor(y_[:].rearrange("p (g c) -> p g c", g=8), y_[:].rearrange("p (g c) -> p g c", g=8),
                                                   yr[:, :, 0:256], ALU.add),
                 reads=yrall + [yk], writes=[yk])
            P.op("dve", lambda e: e.tensor_tensor(yd[:].rearrange("p (r d) -> p r d", d=64), xs3, bc_last(dsk[:], 64), ALU.mult),
                 reads=[xsk, "dsk"], writes=[ydk])
            P.op("pool", lambda e: e.tensor_tensor(y_[:], y_[:], yd[:], ALU.add), reads=[yk, ydk], writes=[yk])
            P.dma("pool", y_d[ts, :], y_[:], reads=[yk], writes=[("y_d", ch)], stream="st")

        P.begin_group()
        pre_a(0)
        P.end_group()
        P.begin_group()
        pre_b(0)
        pre_a(1)
        P.end_group()
        for ch in range(32):
            P.begin_group()
            main(ch)
            if ch + 1 < 32:
                pre_b(ch + 1)
            if ch + 2 < 32:
                pre_a(ch + 2)
            P.end_group()


def phase_ssd_gate(K, c, y_d, hT_d, hT_key, w_in, norm_g, ynT_d):
    P = K.P
    with K.scope():
        wz = load_w_bf16(K, "wz", w_in, 8, 2048, 0)
        ng = load_bcast_row(K, "ng", norm_g, 2048)
        eps_r = K.sb("eps_r", [128, 1], F32)
        P.op("pool", lambda e: e.memset(eps_r[:], RMS_EPS), writes=["eps_r"])
        c["tp_b"] = Pool(K, "tpb", 2, [128, 8, 128], BF16, psum=True)
        c["tp_i"] = 0
        hin = Pool(K, "hin", 2, [128, 8, 512], BF16)
        yin = Pool(K, "yin", 2, [128, 2048], F32)
        pz = Pool(K, "pz", 3, [128, 512], F32, psum=True)
        sz = Pool(K, "sz", 2, [128, 512], F32)
        yg = Pool(K, "yg", 2, [128, 2048], F32)
        junk = K.sb("junk", [128, 2048], F32)
        ss = Pool(K, "ss", 2, [128, 16], F32)
        ynb = Pool(K, "ynb", 2, [128, 2048], BF16)
        yTs = Pool(K, "yTs", 2, [128, 16, 512], BF16)
        hTv = hT_d.rearrange("(k p) t -> p k t", p=128)
        ynTv = ynT_d.rearrange("(k p) t -> p k t", p=128)
        for ch in range(8):
            hi_, hik = hin.next()
            P.dma("sp", hi_[:], hTv[:, :, ch * 512:(ch + 1) * 512], reads=hT_key(ch), writes=[hik], stream="ld")
            ys, ysk = yTs.next()
            for t in range(4):
                if t % 2 == 0:
                    P.begin_group()
                P.next_stream()
                ti = ch * 4 + t
                y_, yk = yin.next()
                P.dma("sp", y_[:], y_d[ti * 128:(ti + 1) * 128, :], reads=[("y_d", ti)], writes=[yk], stream="ld")
                g_, gk = yg.next()
                for blk in range(4):
                    bs = slice(blk * 512, (blk + 1) * 512)
                    pz_, pzk = pz.next()
                    for k in range(8):
                        P.op("pe", lambda e, pz_=pz_, hi_=hi_, k=k, t=t, bs=bs: e.matmul(
                            pz_[:], hi_[:, k, t * 128:(t + 1) * 128], wz[:, k, bs], start=(k == 0), stop=(k == 7)),
                            reads=[hik, ("wz", k)], writes=[pzk])
                    s_, sk = sz.next()
                    P.op("act", lambda e, s_=s_, pz_=pz_: e.activation(s_[:], pz_[:], AF.Silu), reads=[pzk], writes=[sk])
                    P.op("dve", lambda e, g_=g_, y_=y_, s_=s_, bs=bs: e.tensor_tensor(g_[:, bs], y_[:, bs], s_[:], ALU.mult),
                         reads=[yk, sk], writes=[(gk, blk)])
                q_, qk = ss.next()
                P.op("act", lambda e, g_=g_: e.activation(junk[:], g_[:], AF.Square),
                     reads=[(gk, b) for b in range(4)], writes=["junk"])
                P.op("dve", lambda e, q_=q_: e.tensor_reduce(q_[:, 0:8], junk[:].rearrange("p (g d) -> p g d", d=256), AX.X, ALU.add),
                     reads=["junk"], writes=[(qk, g) for g in range(8)])
                qall = [(qk, g) for g in range(8)]
                P.op("act", lambda e, q_=q_: e.activation(q_[:, 8:16], q_[:, 0:8], AF.Sqrt, bias=eps_r[:], scale=1.0 / 256.0),
                     reads=qall + ["eps_r"], writes=[(qk, "s")])
                P.op("dve", lambda e, q_=q_: e.reciprocal(q_[:, 8:16], q_[:, 8:16]), reads=[(qk, "s")], writes=[(qk, "s")])
                P.op("dve", lambda e, g_=g_, q_=q_: e.tensor_tensor(
                    g_[:].rearrange("p (g d) -> p g d", d=256), g_[:].rearrange("p (g d) -> p g d", d=256),
                    bc_last(q_[:, 8:16], 256), ALU.mult),
                    reads=[(gk, b) for b in range(4)] + [(qk, "s")], writes=[(gk, b) for b in range(4)])
                nb, nbk = ynb.next()
                P.op("dve", lambda e, nb=nb, g_=g_: e.tensor_tensor(nb[:], g_[:], ng[:], ALU.mult),
                     reads=[(gk, b) for b in range(4)] + ["ng"], writes=[nbk])
                transpose_to_hT(K, c, nb, nbk, ys, (ysk, t), 16, t * 128)
                if t % 2 == 1:
                    P.end_group()
            for k0 in (0, 8):
                P.dma("pool", ynTv[:, k0:k0 + 8, ch * 512:(ch + 1) * 512], ys[:, k0:k0 + 8, :], reads=[(ysk, t) for t in range(4)],
                      writes=[("ynT_d", ch, k0)], stream="st")


def phase_router(K, c, h1_d, h1_key, router, gates):
    P = K.P
    with K.scope():
        rt = K.sb("rt", [128, 8, 8], F32)
        P.dma("sp", rt[:], router.rearrange("(k p) e -> p k e", p=128), writes=["rt"], stream="cst")
        hin = Pool(K, "rhin", 4, [128, D], F32)
        tpf = Pool(K, "tpf", 4, [128, 4, 128], F32, psum=True)
        hTf = Pool(K, "hTf", 4, [128, 8, 128], F32)
        plg = Pool(K, "plg", 4, [128, 512], F32, psum=True)
        sm = Pool(K, "rsm", 4, [128, 8, 8], F32)
        for ti in range(32):
            if ti % 4 == 0:
                P.begin_group()
            P.next_stream()
            h_, hk = hin.next()
            P.dma("sp", h_[:], h1_d[ti * 128:(ti + 1) * 128, :], reads=h1_key(ti), writes=[hk], stream="ld")
            hT_, hTk = hTf.next()
            for half in range(2):
                pt, ptk = tpf.next()
                for k in range(4):
                    kk = half * 4 + k
                    P.op("pe", lambda e, pt=pt, k=k, kk=kk, h_=h_: e.transpose(pt[:, k, :], h_[:, kk * 128:(kk + 1) * 128], c["ident_f"][:]),
                         reads=[hk, "ident_f"], writes=[ptk])
                if half == 0:
                    P.op("act", lambda e, hT_=hT_, pt=pt: e.copy(hT_[:, 0:4, :], pt[:]), reads=[ptk], writes=[(hTk, 0)])
                else:
                    P.op("dve", lambda e, hT_=hT_, pt=pt: e.tensor_copy(hT_[:, 4:8, :], pt[:]), reads=[ptk], writes=[(hTk, 1)])
            pl, plk = plg.next()
            for k in range(8):
                P.op("pe", lambda e, pl=pl, hT_=hT_, k=k: e.matmul(pl[:, 0:8], hT_[:, k, :], rt[:, k, :], start=(k == 0), stop=(k == 7)),
                     reads=[(hTk, k // 4), "rt"], writes=[plk])
            s_, sk = sm.next()
            lg, m8, ex, msk, den = (s_[:, q, :] for q in range(5))
            P.op("act", lambda e, lg=lg, pl=pl: e.copy(lg, pl[:, 0:8]), reads=[plk], writes=[(sk, "lg")])
            P.op("dve", lambda e, m8=m8, lg=lg: e.max(m8, lg), reads=[(sk, "lg")], writes=[(sk, "m8")])
            P.op("dve", lambda e, den=den, m8=m8: e.tensor_scalar(den[:, 1:2], m8[:, 0:1], -1.0, None, ALU.mult),
                 reads=[(sk, "m8")], writes=[(sk, "nm")])
            P.op("act", lambda e, ex=ex, lg=lg, den=den: e.activation(ex, lg, AF.Exp, bias=den[:, 1:2], scale=1.0),
                 reads=[(sk, "lg"), (sk, "nm")], writes=[(sk, "ex")])
            P.op("dve", lambda e, msk=msk, lg=lg, m8=m8: e.tensor_scalar(msk, lg, m8[:, 1:2], None, ALU.is_ge),
                 reads=[(sk, "lg"), (sk, "m8")], writes=[(sk, "msk")])
            P.op("dve", lambda e, ex=ex, msk=msk: e.tensor_tensor(ex, ex, msk, ALU.mult),
                 reads=[(sk, "ex"), (sk, "msk")], writes=[(sk, "ex")])
            P.op("dve", lambda e, den=den, ex=ex: e.reduce_sum(den[:, 0:1], ex, AX.X), reads=[(sk, "ex")], writes=[(sk, "den")])
            P.op("dve", lambda e, den=den: e.reciprocal(den[:, 2:3], den[:, 0:1]), reads=[(sk, "den")], writes=[(sk, "rden")])
            P.op("dve", lambda e, ex=ex, den=den, ti=ti: e.tensor_scalar(gates[:, ti, :], ex, den[:, 2:3], None, ALU.mult),
                 reads=[(sk, "ex"), (sk, "rden")], writes=[("gates", ti)])
            if ti % 4 == 3:
                P.end_group()


def phase_moe(K, c, h1T_d, h1T_key, w_g, w_u, w_d, gates, ffn_d):
    P = K.P
    TB = 2048
    NTB = TB // 128
    JG = 4
    NG = EDIM // (128 * JG)
    with K.scope():
        hin = Pool(K, "mhin", 1, [128, 8, TB], BF16)
        acc = K.sb("macc", [128, NTB, D], F32)
        wg = Pool(K, "mwg", 2, [128, 8, 512], BF16)
        wu = Pool(K, "mwu", 2, [128, 8, 512], BF16)
        wd = Pool(K, "mwd", 2, [128, JG, D], BF16)
        actT = Pool(K, "mact", 1, [128, JG, TB], BF16)
        sgt = Pool(K, "msg", 2, [128, 512], F32)
        pgu = Pool(K, "mpgu", 2, [128, 2, 512], F32, psum=True)
        pdn = Pool(K, "mpdn", 2, [128, 2, 512], F32, psum=True)
        h1Tv = h1T_d.rearrange("(k p) t -> p k t", p=128)
        for blk in range(S // TB):
            hi_, hik = hin.next()
            for q in range(TB // 512):
                P.dma("sp", hi_[:, :, q * 512:(q + 1) * 512], h1Tv[:, :, blk * TB + q * 512: blk * TB + (q + 1) * 512],
                      reads=h1T_key((blk * TB) // 512 + q), writes=[(hik, q)], stream="ld")
            first = True
            for ex in range(NE):
                wgr = w_g[ex].rearrange("(k p) c -> p k c", p=128)
                wur = w_u[ex].rearrange("(k p) c -> p k c", p=128)
                wdr = w_d[ex].rearrange("(j p) c -> p j c", p=128)
                for jg in range(NG):
                    wg_, wgk = wg.next()
                    wu_, wuk = wu.next()
                    wd_, wdk = wd.next()
                    cs = slice(jg * 512, (jg + 1) * 512)
                    for k0 in (0, 4):
                        P.dma("pool", wg_[:, k0:k0 + 4, :], wgr[:, k0:k0 + 4, cs], writes=[(wgk, k0)], stream="wc")
                        P.dma("pool", wu_[:, k0:k0 + 4, :], wur[:, k0:k0 + 4, cs], writes=[(wuk, k0)], stream="wc")
                    P.dma("pool", wd_[:], wdr[:, jg * JG:(jg + 1) * JG, :], writes=[wdk], stream="wc")
                    at, atk = actT.next()
                    for q in range(TB // 512):
                        qs = slice(q * 512, (q + 1) * 512)
                        for jj in range(JG):
                            pg_, pgk_ = pgu.next()
                            for gu, (w_, wk_) in enumerate(((wg_, wgk), (wu_, wuk))):
                                for k in range(8):
                                    P.op("pe", lambda e, pg_=pg_, w_=w_, gu=gu, k=k, jj=jj, hi_=hi_, qs=qs: e.matmul(
                                        pg_[:, gu, :], w_[:, k, jj * 128:(jj + 1) * 128], hi_[:, k, qs],
                                        start=(k == 0), stop=(k == 7)),
                                        reads=[(wk_, (k // 4) * 4), (hik, q)], writes=[(pgk_, gu)])
                            sg_, sgk_ = sgt.next()
                            P.op("act", lambda e, sg_=sg_, pg_=pg_: e.activation(sg_[:], pg_[:, 0, :], AF.Silu),
                                 reads=[(pgk_, 0)], writes=[sgk_])
                            P.op("dve", lambda e, at=at, jj=jj, qs=qs, sg_=sg_, pg_=pg_: e.tensor_tensor(
                                at[:, jj, qs], sg_[:], pg_[:, 1, :], ALU.mult),
                                reads=[sgk_, (pgk_, 1)], writes=[(atk, jj, q)])
                    for t in range(NTB):
                        ti = blk * NTB + t
                        pd_, pdk = pdn.next()
                        for half in range(2):
                            for jj in range(JG):
                                P.op("pe", lambda e, pd_=pd_, at=at, jj=jj, t=t, half=half, wd_=wd_: e.matmul(
                                    pd_[:, half, :], at[:, jj, t * 128:(t + 1) * 128], wd_[:, jj, half * 512:(half + 1) * 512],
                                    start=(jj == 0), stop=(jj == JG - 1)),
                                    reads=[(atk, jj, t // 4), wdk], writes=[(pdk, half)])
                        src = pd_[:].rearrange("p a b -> p (a b)")
                        if first:
                            P.op("dve", lambda e, t=t, src=src, ti=ti, ex=ex: e.tensor_scalar(
                                acc[:, t, :], src, gates[:, ti, ex:ex + 1], None, ALU.mult),
                                reads=[(pdk, 0), (pdk, 1), ("gates", ti)], writes=[("macc", t)])
                        else:
                            P.op("dve", lambda e, t=t, src=src, ti=ti, ex=ex: e.scalar_tensor_tensor(
                                acc[:, t, :], src, gates[:, ti, ex:ex + 1], acc[:, t, :], ALU.mult, ALU.add),
                                reads=[(pdk, 0), (pdk, 1), ("gates", ti), ("macc", t)], writes=[("macc", t)])
                    first = False
            for t in range(NTB):
                ti = blk * NTB + t
                P.dma("sp", ffn_d[ti * 128:(ti + 1) * 128, :], acc[:, t, :], reads=[("macc", t)], writes=[("ffn_d", ti)], stream="st")


def phase_epilogue(K, c, h1_d, h1_key, ffn_d, ffn_key, g_row, b_row, w_pg, w_pp, p_d, out_d):
    P = K.P
    with K.scope():
        g_bc, b_bc, wpg, wpp = epilogue_setup(K, c, g_row, b_row, w_pg, w_pp, npg=2, nb=4)
        h1in = Pool(K, "h1in", 4, [128, D], F32)
        fin = Pool(K, "fin", 4, [128, D], F32)
        zt = Pool(K, "zt", 4, [128, D], F32)
        ctxs = {}

        def s1(ti):
            ts = slice(ti * 128, (ti + 1) * 128)
            h_, hk = h1in.next()
            f_, fk = fin.next()
            P.dma("sp", h_[:], h1_d[ts, :], reads=h1_key(ti), writes=[hk], stream="ld")
            P.dma("sp", f_[:], ffn_d[ts, :], reads=ffn_key(ti), writes=[fk], stream="ld")
            z, zk = zt.next()
            P.op("dve", lambda e: e.scalar_tensor_tensor(z[:], h_[:], ALPHA, f_[:], ALU.mult, ALU.add),
                 reads=[hk, fk], writes=[zk])
            ctxs[ti] = epilogue_s1(K, c, z, zk, ti * 128, g_bc, b_bc, p_d)

        for g0 in range(0, 32, 4):
            P.begin_group()
            for ti in range(g0, g0 + 4):
                P.next_stream()
                s1(ti)
                epilogue_s2(K, c, ctxs.pop(ti), wpg, wpp, out_d, "out_d", None)
            P.end_group()


def build(mode="full", upto=99):
    K = KB()
    nc = K.nc
    P = K.P
    ein = lambda name, shape: nc.dram_tensor(name, shape, F32, kind="ExternalInput").ap()
    p_d = ein("p", [2, S, 256])
    ln_mix_g = ein("ln_mix_g", [2, D]); ln_mix_b = ein("ln_mix_b", [2, D])
    ln_ffn_g = ein("ln_ffn_g", [2, D]); ln_ffn_b = ein("ln_ffn_b", [2, D])
    ple_w_proj = ein("ple_w_proj", [2, 256, D]); ple_w_gate = ein("ple_w_gate", [2, D, D])
    c = make_consts(K)
    c["eps_ln"] = K.sb("eps_ln", [128, 1], F32)
    P.op("pool", lambda e: e.memset(c["eps_ln"][:], LN_EPS), writes=["eps_ln"])
    c["tp_eng"] = ("act", "dve")
    c["tp_i"] = 0
    if mode in ("l0", "full"):
        x_d = ein("x", [S, D])
        fox_w_in = ein("fox_w_in", [D, 3088]); fox_b_f = ein("fox_b_f", [16]); fox_w_o = ein("fox_w_o", [D, D])
        ffn_w_gate = ein("ffn_w_gate", [D, FFN]); ffn_w_up = ein("ffn_w_up", [D, FFN]); ffn_w_down = ein("ffn_w_down", [FFN, D])
        qaT = K.dram("qaT", [16, 66, S], BF16)
        kaT = K.dram("kaT", [16, 66, S], BF16)
        vaug = K.dram("vaug", [S, 16, 128], BF16)
        attnT = K.dram("attnT", [D, S], BF16)
        h1 = K.dram("l0_h1", [S, D], F32)
        h1T = K.dram("l0_h1T", [D, S], BF16)
        ctok = K.sb("ctok", [128, 32, 16], F32)
        cref = K.sb("cref", [128, 8, 16], F32)
        if mode == "l0":
            h3_d = nc.dram_tensor("out", [S, D], F32, kind="ExternalOutput").ap()
            h3T_d = nc.dram_tensor("outT", [D, S], BF16, kind="ExternalOutput").ap()
        else:
            h3_d = K.dram("l0_h3", [S, D], F32)
            h3T_d = K.dram("l0_h3T", [D, S], BF16)
        phase_qkv(K, c, x_d, fox_w_in, fox_b_f, qaT, kaT, vaug, ctok, cref)
        phase_attn(K, c, qaT, kaT, vaug, ctok, cref, attnT)
        phase_proj_ln(K, c, attnT, lambda ch: [("attn_d", ch)], 8, fox_w_o, x_d, lambda t: [],
                      ln_mix_g[0], ln_mix_b[0], h1, h1T, "l0")
        actT_d = K.dram("l0_actT", [FFN, S], BF16)
        phase_ffn(K, c, h1, h1T, ffn_w_gate, ffn_w_up, ffn_w_down, ln_ffn_g[0], ln_ffn_b[0],
                  ple_w_gate[0], ple_w_proj[0], p_d[0], h3_d, h3T_d, actT_d)
        h3_key = lambda ti: [("out_d", ti)]
        h3T_key = lambda ch: [("outT_d", ch)]
    if mode == "l1":
        h3_d = ein("h", [S, D])
        h3T_d = nc.dram_tensor("hT", [D, S], BF16, kind="ExternalInput").ap()
        h3_key = lambda ti: []
        h3T_key = lambda ch: []
    if mode in ("l1", "full"):
        ssd_w_in = ein("ssd_w_in", [D, 6176]); ssd_conv_w = ein("ssd_conv_w", [4, 4096]); ssd_conv_b = ein("ssd_conv_b", [4096])
        ssd_dt_bias = ein("ssd_dt_bias", [32]); ssd_a_log = ein("ssd_a_log", [32]); ssd_d = ein("ssd_d", [32])
        ssd_norm_g = ein("ssd_norm_g", [2048]); ssd_w_out = ein("ssd_w_out", [2048, D])
        moe_router = ein("moe_router", [D, NE]); moe_w_gate = ein("moe_w_gate", [NE, D, EDIM])
        moe_w_up = ein("moe_w_up", [NE, D, EDIM]); moe_w_down = ein("moe_w_down", [NE, EDIM, D])
        out_d = nc.dram_tensor("out", [S, D], F32, kind="ExternalOutput").ap()
        xs_d = K.dram("xs_d", [S, 2048], F32)
        Btok_d = K.dram("Btok_d", [S, 1024], BF16)
        BT_d = K.dram("BT_d", [8, 128, S], BF16)
        CT_d = K.dram("CT_d", [8, 128, S], BF16)
        y_d = K.dram("y_d", [S, 2048], F32)
        ynT_d = K.dram("ynT_d", [2048, S], BF16)
        g1 = K.dram("l1_h1", [S, D], F32)
        g1T = K.dram("l1_h1T", [D, S], BF16)
        ffn_d = K.dram("ffn_d", [S, D], F32)
        dt_tok = K.sb("dt_tok", [128, 32, 32], F32)
        gates = K.sb("gates", [128, 32, NE], F32)
        phase_ssd_proj(K, c, h3T_d, h3T_key, ssd_w_in, ssd_conv_w, ssd_conv_b, ssd_dt_bias, xs_d, Btok_d, BT_d, CT_d, dt_tok)
        if upto >= 2:
            phase_ssd_core(K, c, xs_d, Btok_d, BT_d, CT_d, dt_tok, ssd_a_log, ssd_d, y_d)
        if upto >= 3:
            phase_ssd_gate(K, c, y_d, h3T_d, h3T_key, ssd_w_in, ssd_norm_g, ynT_d)
        if upto >= 4:
            phase_proj_ln(K, c, ynT_d, lambda ch: [("ynT_d", ch, 0), ("ynT_d", ch, 8)], 16, ssd_w_out, h3_d, h3_key,
                          ln_mix_g[1], ln_mix_b[1], g1, g1T, "l1")
        if upto >= 5:
            phase_router(K, c, g1, lambda ti: [("l1h1_d", ti)], moe_router, gates)
        if upto >= 6:
            phase_moe(K, c, g1T, lambda ch: [("l1h1T_d", ch)], moe_w_gate, moe_w_up, moe_w_down, gates, ffn_d)
        if upto >= 7:
            phase_epilogue(K, c, g1, lambda ti: [("l1h1_d", ti)], ffn_d, lambda ti: [("ffn_d", ti)],
                           ln_ffn_g[1], ln_ffn_b[1], ple_w_gate[1], ple_w_proj[1], p_d[1], out_d)
        if upto < 7:
            dbg = {1: xs_d, 2: y_d, 3: None, 4: g1, 5: g1, 6: ffn_d}[upto]
            if dbg is not None:
                dbg_o = nc.dram_tensor("dbg", list(dbg.shape), F32, kind="ExternalOutput").ap()
                P.dma("sp", dbg_o, dbg, reads=[k for k in P.lastw if isinstance(k, tuple) and k[0] in ("xs_d", "y_d", "l1h1_d", "ffn_d")],
                      writes=["dbg_o"], stream="st")
    P.flush(barrier=True)
    K.root.close()
    return nc, P


FUSED = True

_COMMON = ["ln_mix_g", "ln_mix_b", "ln_ffn_g", "ln_ffn_b", "ple_w_proj", "ple_w_gate"]
_L0 = ["fox_w_in", "fox_b_f", "fox_w_o", "ffn_w_gate", "ffn_w_up", "ffn_w_down"]
_L1 = ["ssd_w_in", "ssd_conv_w", "ssd_conv_b", "ssd_dt_bias", "ssd_a_log", "ssd_d", "ssd_norm_g", "ssd_w_out",
       "moe_router", "moe_w_gate", "moe_w_up", "moe_w_down"]


def kernel(**inputs):
    f = lambda a: np.ascontiguousarray(np.asarray(a, dtype=np.float32))
    x = f(inputs["x"])
    p = f(inputs["p"])
    B = x.shape[0]
    common = {k: f(inputs[k]) for k in _COMMON}
    l0 = {k: f(inputs[k])[0] for k in _L0}
    l1 = {k: f(inputs[k])[0] for k in _L1}
    cores = list(range(B))
    if FUSED:
        nc, _ = build("full")
        maps = [dict(common, **l0, **l1, x=x[b], p=np.ascontiguousarray(p[:, b])) for b in cores]
        res = run_bass_kernel_spmd(nc, maps, core_ids=cores)
        return np.stack([np.asarray(r["out"], dtype=np.float32) for r in res.results], axis=0)
    nc0, _ = build("l0")
    maps = [dict(common, **l0, x=x[b], p=np.ascontiguousarray(p[:, b])) for b in cores]
    r0 = run_bass_kernel_spmd(nc0, maps, core_ids=cores).results
    nc1, _ = build("l1")
    maps = [dict(common, **l1, h=r0[b]["out"], hT=r0[b]["outT"], p=np.ascontiguousarray(p[:, b])) for b in cores]
    r1 = run_bass_kernel_spmd(nc1, maps, core_ids=cores).results
    return np.stack([np.asarray(r["out"], dtype=np.float32) for r in r1], axis=0)
```

```python
import numpy as np
from contextlib import ExitStack
import concourse.bass as bass
import concourse.mybir as mybir
from concourse.bass_utils import run_bass_kernel_spmd

F32 = mybir.dt.float32
BF16 = mybir.dt.bfloat16
AF = mybir.ActivationFunctionType
ALU = mybir.AluOpType
AX = mybir.AxisListType

S = 4096
D = 1024
NT = S // 128
ALPHA = (2.0 * 2) ** 0.25
LN_EPS = 1e-5
RMS_EPS = 1e-5
FFN = 2816
NE = 8
EDIM = 3584
COMPUTE = ("pe", "act", "dve", "pool")


class _Op:
    __slots__ = ("eng", "fn", "is_dma", "stream", "waits", "signal", "sig_sem", "sig_val", "idx")

    def __init__(self, eng, fn, is_dma, stream):
        self.eng = eng
        self.fn = fn
        self.is_dma = is_dma
        self.stream = stream
        self.waits = []
        self.signal = is_dma
        self.sig_sem = None
        self.sig_val = 0


class Prog:
    def __init__(self, nc, stack, nstream_sems=6):
        self.nc = nc
        self.stack = stack
        self.ops = []
        self.emitted = 0
        self.lastw = {}
        self.readers = {}
        self.engs = {"pe": nc.tensor, "act": nc.scalar, "dve": nc.vector,
                     "pool": nc.gpsimd, "sp": nc.sync}
        self.sem = {e: stack.enter_context(nc.semaphore("s_" + e)) for e in COMPUTE}
        self.cnt = {e: 0 for e in COMPUTE}
        self.st_sems = {}
        self.st_cnt = {}
        self.waited = {e: {} for e in self.engs}
        self.K = nstream_sems
        self.n_inst = {e: 0 for e in self.engs}
        self._grp = None
        self._cur = None

    def begin_group(self):
        self._grp = []
        self._cur = None

    def next_stream(self):
        self._cur = []
        self._grp.append(self._cur)

    def end_group(self):
        streams = [st for st in self._grp if st]
        self._grp = None
        self._cur = None
        n = len(streams)
        pw = [dict() for _ in range(n)]
        pa = [dict() for _ in range(n)]
        for si, st in enumerate(streams):
            for (eng, fn, reads, writes, is_dma, stream) in st:
                for k in reads:
                    pa[si][k] = pa[si].get(k, 0) + 1
                for k in writes:
                    pa[si][k] = pa[si].get(k, 0) + 1
                    pw[si][k] = pw[si].get(k, 0) + 1
        idx = [0] * n
        left = sum(len(st) for st in streams)
        while left:
            for si in range(n):
                if idx[si] >= len(streams[si]):
                    continue
                eng, fn, reads, writes, is_dma, stream = streams[si][idx[si]]
                ok = True
                for k in reads:
                    for s2 in range(si):
                        if pw[s2].get(k, 0) > 0:
                            ok = False
                            break
                    if not ok:
                        break
                if ok:
                    for k in writes:
                        for s2 in range(si):
                            if pa[s2].get(k, 0) > 0:
                                ok = False
                                break
                        if not ok:
                            break
                if not ok:
                    continue
                self._add(eng, fn, reads, writes, is_dma, stream)
                for k in reads:
                    pa[si][k] -= 1
                for k in writes:
                    pa[si][k] -= 1
                    pw[si][k] -= 1
                idx[si] += 1
                left -= 1

    def _add(self, eng, fn, reads, writes, is_dma=False, stream=None):
        if self._cur is not None:
            self._cur.append((eng, fn, tuple(reads), tuple(writes), is_dma, stream))
            return None
        op = _Op(eng, fn, is_dma, stream)
        op.idx = len(self.ops)
        deps = {}
        for k in reads:
            w = self.lastw.get(k)
            if w is not None:
                deps[w] = deps.get(w, "") + "r"
        for k in writes:
            w = self.lastw.get(k)
            if w is not None:
                deps[w] = deps.get(w, "") + "w"
            for r in self.readers.get(k, ()):
                deps[r] = deps.get(r, "") + "a"
        for a_idx, kinds in deps.items():
            a = self.ops[a_idx]
            need = True
            if not a.is_dma and not is_dma and a.eng == eng:
                need = ("r" in kinds) and eng != "pe"
            if need:
                op.waits.append(a_idx)
                a.signal = True
        for k in reads:
            self.readers.setdefault(k, []).append(op.idx)
        for k in writes:
            self.lastw[k] = op.idx
            self.readers[k] = []
        self.ops.append(op)
        return op

    def op(self, eng, fn, reads=(), writes=()):
        return self._add(eng, fn, reads, writes)

    def dma(self, queue, out, in_, reads=(), writes=(), stream="ld", **kw):
        def fn(e, out=out, in_=in_, kw=kw):
            return e.dma_start(out=out, in_=in_, **kw)
        return self._add(queue, fn, reads, writes, is_dma=True, stream=stream)

    def _wait(self, e, s, v):
        key = id(s)
        if self.waited[e].get(key, -1) >= v:
            return
        self.engs[e].wait_ge(s, v)
        self.n_inst[e] += 1
        self.waited[e][key] = v

    def flush(self, barrier=True):
        nc = self.nc
        pend = range(self.emitted, len(self.ops))
        live = set(self.lastw.values())
        for rs in self.readers.values():
            live.update(rs)
        last_on = {}
        for i in pend:
            op = self.ops[i]
            if i in live:
                op.signal = True
            if not op.is_dma:
                last_on[op.eng] = i
        for i in last_on.values():
            self.ops[i].signal = True
        for i in pend:
            op = self.ops[i]
            e = op.eng
            for a_idx in op.waits:
                a = self.ops[a_idx]
                self._wait(e, a.sig_sem, a.sig_val)
            if op.is_dma:
                st = op.stream
                if st not in self.st_sems:
                    self.st_sems[st] = [self.stack.enter_context(nc.semaphore("d_%s_%d" % (st, j)))
                                        for j in range(self.K)]
                    self.st_cnt[st] = 0
                n = self.st_cnt[st]
                self.st_cnt[st] += 1
                s = self.st_sems[st][n % self.K]
                if n >= self.K:
                    self._wait(e, s, 16 * (n // self.K))
                ins = op.fn(self.engs[e])
                ins.then_inc(s, 16)
                op.sig_sem = s
                op.sig_val = 16 * (n // self.K + 1)
            else:
                ins = op.fn(self.engs[e])
                if op.signal:
                    self.cnt[e] += 1
                    ins.then_inc(self.sem[e], 1)
                    op.sig_sem = self.sem[e]
                    op.sig_val = self.cnt[e]
            self.n_inst[e] += 1
            op.fn = None
        self.emitted = len(self.ops)
        if barrier:
            for e in self.engs:
                for st, sems in self.st_sems.items():
                    n = self.st_cnt[st]
                    for j, s in enumerate(sems):
                        nj = (n - j + self.K - 1) // self.K if n > j else 0
                        if nj > 0:
                            self._wait(e, s, 16 * nj)
                for c in COMPUTE:
                    if c != e and self.cnt[c] > 0:
                        self._wait(e, self.sem[c], self.cnt[c])


class Pool:
    def __init__(self, K, name, n, shape, dt, psum=False):
        self.name = name
        self.n = n
        self.i = 0
        alloc = K.ps if psum else K.sb
        self.t = [alloc("%s%d" % (name, j), shape, dt) for j in range(n)]

    def next(self):
        j = self.i % self.n
        self.i += 1
        return self.t[j], (self.name, j)


class KB:
    def __init__(self):
        self.nc = bass.Bass("TRN2", target_bir_lowering=False)
        self.root = ExitStack()
        self.stacks = [self.root]
        self.P = Prog(self.nc, self.root)
        self._uid = 0

    def sb(self, name, shape, dt):
        self._uid += 1
        return self.stacks[-1].enter_context(self.nc.sbuf_tensor("%s_%d" % (name, self._uid), shape, dt))

    def ps(self, name, shape, dt):
        self._uid += 1
        return self.stacks[-1].enter_context(self.nc.psum_tensor("%s_%d" % (name, self._uid), shape, dt))

    def dram(self, name, shape, dt, kind="Internal"):
        return self.nc.dram_tensor(name, shape, dt, kind=kind).ap()

    class _Scope:
        def __init__(self, K):
            self.K = K

        def __enter__(self):
            st = ExitStack()
            self.K.stacks.append(st)
            return st

        def __exit__(self, *a):
            if a[0] is None:
                self.K.P.flush(barrier=True)
            st = self.K.stacks.pop()
            st.close()
            return False

    def scope(self):
        return KB._Scope(self)


def make_consts(K):
    P = K.P
    c = {}
    c["ident_f"] = K.sb("ident_f", [128, 128], F32)
    c["ident_b"] = K.sb("ident_b", [128, 128], BF16)
    idf, idb = c["ident_f"], c["ident_b"]
    P.op("pool", lambda e: e.memset(idf[:], 0.0), writes=["ident_f"])
    P.op("pool", lambda e: e.affine_select(idf[:], idf[:], [[-1, 128]], ALU.not_equal, 1.0,
                                           base=0, channel_multiplier=1),
         reads=["ident_f"], writes=["ident_f"])
    P.op("dve", lambda e: e.tensor_copy(idb[:], idf[:]), reads=["ident_f"], writes=["ident_b"])
    return c


def load_bcast_row(K, name, dram_row, n, queue="sp"):
    t = K.sb(name, [128, n], F32)
    K.P.dma(queue, t[:], dram_row.partition_broadcast(128), writes=[name], stream="cst")
    return t


def load_w_bf16(K, name, w_dram, kchunks, cols, c0=0, key=None):
    t = K.sb(name, [128, kchunks, cols], BF16)
    wr = w_dram.rearrange("(k p) c -> p k c", p=128)
    for k in range(kchunks):
        K.P.dma("pool", t[:, k, :], wr[:, k, c0:c0 + cols], writes=[(key or name, k)], stream="wc")
    return t


def ln_tile(K, c, zt, zk, g_bc, b_bc, gk, bk_, out_t, out_k):
    P = K.P
    st, stk = c["ln_stats"].next()
    mv, mvk = c["ln_mv"].next()
    for i in range(2):
        P.op("dve", lambda e, i=i: e.bn_stats(st[:, i, :], zt[:, i * 512:(i + 1) * 512]),
             reads=[zk], writes=[stk])
    P.op("dve", lambda e: e.bn_aggr(mv[:, 0:2], st[:].rearrange("p a b -> p (a b)")), reads=[stk], writes=[mvk])
    P.op("act", lambda e: e.activation(mv[:, 2:3], mv[:, 1:2], AF.Sqrt, bias=c["eps_ln"][:], scale=1.0),
         reads=[mvk, "eps_ln"], writes=[mvk])
    P.op("dve", lambda e: e.reciprocal(mv[:, 3:4], mv[:, 2:3]), reads=[mvk], writes=[mvk])
    P.op("dve", lambda e: e.scalar_tensor_tensor(mv[:, 4:5], mv[:, 0:1], -1.0, mv[:, 3:4], ALU.mult, ALU.mult),
         reads=[mvk], writes=[mvk])
    P.op("act", lambda e: e.activation(out_t, zt, AF.Identity, bias=mv[:, 4:5], scale=mv[:, 3:4]),
         reads=[zk, mvk], writes=[out_k])
    P.op("dve", lambda e: e.tensor_tensor(out_t, out_t, g_bc, ALU.mult), reads=[out_k, gk], writes=[out_k])
    P.op("pool", lambda e: e.tensor_tensor(out_t, out_t, b_bc, ALU.add), reads=[out_k, bk_], writes=[out_k])


def transpose_to_hT(K, c, src_bf, src_k, dst, dst_k, ncol_chunks=8, dst_off=0):
    P = K.P
    for half in range(0, ncol_chunks, 8):
        n = min(8, ncol_chunks - half)
        pt, ptk = c["tp_b"].next()
        for k in range(n):
            P.op("pe", lambda e, k=k, pt=pt, half=half: e.transpose(pt[:, k, :], src_bf[:, (half + k) * 128:(half + k + 1) * 128],
                                                  c["ident_b"][:]),
                 reads=[src_k, "ident_b"], writes=[ptk])
        eng = c["tp_eng"][c["tp_i"] % 2]
        c["tp_i"] += 1
        if eng == "act":
            P.op("act", lambda e, pt=pt, half=half, n=n: e.copy(dst[:, half:half + n, dst_off:dst_off + 128], pt[:, 0:n, :]),
                 reads=[ptk], writes=[dst_k])
        else:
            P.op("dve", lambda e, pt=pt, half=half, n=n: e.tensor_copy(dst[:, half:half + n, dst_off:dst_off + 128], pt[:, 0:n, :]),
                 reads=[ptk], writes=[dst_k])


def phase_qkv(K, c, x_d, w_in, b_f, qaT, kaT, vaug, ctok, cref):
    P = K.P
    with K.scope():
        wqk = load_w_bf16(K, "wqk", w_in, 8, 2048, 0)
        wv = load_w_bf16(K, "wv", w_in, 8, 1024, 2048)
        wf = load_w_bf16(K, "wf", w_in, 8, 16, 3072)
        negb = K.sb("negb", [16, 1], F32)
        P.dma("sp", negb[:], b_f.rearrange("(h o) -> h o", o=1), writes=["negb"], stream="cst")
        P.op("dve", lambda e: e.tensor_scalar(negb[:], negb[:], -1.0, None, ALU.mult), reads=["negb"], writes=["negb"])
        nl = K.sb("nl", [16, S], F32)
        cn = K.sb("cn", [16, S], F32)
        ones16 = K.sb("ones16", [16, 512], F32)
        P.op("pool", lambda e: e.memset(ones16[:], 1.0), writes=["ones16"])
        xin = Pool(K, "xin", 4, [128, D], F32)
        xT = Pool(K, "xT", 2, [128, 8, 512], BF16)
        tpf = Pool(K, "tpf", 2, [128, 4, 128], F32, psum=True)
        pqk = Pool(K, "pqk", 3, [128, 512], F32, psum=True)
        pv = Pool(K, "pv", 2, [128, 512], F32, psum=True)
        pfl = K.ps("pfl", [16, 512], F32)
        qst = Pool(K, "qst", 4, [128, 512], BF16)
        vst = Pool(K, "vst", 2, [128, 16, 128], BF16)
        for j in range(2):
            P.op("pool", lambda e, j=j: e.memset(vst.t[j][:], 1.0), writes=[("vst", j)])
        e1 = K.sb("e1", [16, 512], F32)
        ev = 0
        for ch in range(8):
            xTc, xTk = xT.next()
            for t in range(4):
                tok0 = ch * 512 + t * 128
                xt, xk = xin.next()
                P.dma("sp", xt[:], x_d[tok0:tok0 + 128, :], writes=[xk], stream="ld")
                for half in range(2):
                    pt, ptk = tpf.next()
                    for k in range(4):
                        kk = half * 4 + k
                        P.op("pe", lambda e, k=k, kk=kk, pt=pt, xt=xt: e.transpose(
                            pt[:, k, :], xt[:, kk * 128:(kk + 1) * 128], c["ident_f"][:]),
                            reads=[xk, "ident_f"], writes=[ptk])
                    if half == 0:
                        P.op("act", lambda e, pt=pt, xTc=xTc, t=t: e.copy(xTc[:, 0:4, t * 128:(t + 1) * 128], pt[:]),
                             reads=[ptk], writes=[xTk])
                    else:
                        P.op("dve", lambda e, pt=pt, xTc=xTc, t=t: e.tensor_copy(xTc[:, 4:8, t * 128:(t + 1) * 128], pt[:]),
                             reads=[ptk], writes=[xTk])
            for cc in range(16):
                pq, pqk_k = pqk.next()
                for k in range(8):
                    P.op("pe", lambda e, k=k, cc=cc, pq=pq, xTc=xTc: e.matmul(
                        pq[:], wqk[:, k, cc * 128:(cc + 1) * 128], xTc[:, k, :], start=(k == 0), stop=(k == 7)),
                        reads=[("wqk", k), xTk], writes=[pqk_k])
                qs, qsk = qst.next()
                if ev % 2 == 0:
                    P.op("act", lambda e, qs=qs, pq=pq: e.copy(qs[:], pq[:]), reads=[pqk_k], writes=[qsk])
                else:
                    P.op("dve", lambda e, qs=qs, pq=pq: e.tensor_copy(qs[:], pq[:]), reads=[pqk_k], writes=[qsk])
                ev += 1
                dst = qaT if cc < 8 else kaT
                hp = (cc % 8) * 2
                for hh in range(2):
                    P.dma("sp", dst[hp + hh, 0:64, ch * 512:(ch + 1) * 512], qs[hh * 64:(hh + 1) * 64, :],
                          reads=[qsk], writes=[("qk_d", cc < 8, hp + hh)], stream="st")
            for t in range(4):
                tok0 = ch * 512 + t * 128
                vs, vsk = vst.next()
                for half in range(2):
                    pvt, pvk = pv.next()
                    for k in range(8):
                        P.op("pe", lambda e, k=k, half=half, pvt=pvt, xTc=xTc, t=t: e.matmul(
                            pvt[:], xTc[:, k, t * 128:(t + 1) * 128], wv[:, k, half * 512:(half + 1) * 512],
                            start=(k == 0), stop=(k == 7)),
                            reads=[("wv", k), xTk], writes=[pvk])
                    src = pvt[:].rearrange("p (h d) -> p h d", d=64)
                    if half == 0:
                        P.op("act", lambda e, vs=vs, src=src: e.copy(vs[:, 0:8, 0:64], src), reads=[pvk], writes=[vsk])
                    else:
                        P.op("dve", lambda e, vs=vs, src=src: e.tensor_copy(vs[:, 8:16, 0:64], src), reads=[pvk], writes=[vsk])
                P.dma("sp", vaug[tok0:tok0 + 128, :, :], vs[:], reads=[vsk], writes=[("vaug_d", tok0 // 128)], stream="st")
            for k in range(8):
                P.op("pe", lambda e, k=k, xTc=xTc: e.matmul(pfl[:], wf[:, k, :], xTc[:, k, :], start=(k == 0), stop=(k == 7)),
                     reads=[("wf", k), xTk], writes=["pfl"])
            P.op("act", lambda e: e.activation(e1[:], pfl[:], AF.Exp, bias=negb[:], scale=-1.0),
                 reads=["pfl", "negb"], writes=["e1"])
            P.op("act", lambda e, ch=ch: e.activation(nl[:, ch * 512:(ch + 1) * 512], e1[:], AF.Ln, bias=1.0, scale=1.0),
                 reads=["e1"], writes=[("nl", ch)])
            init = 0.0 if ch == 0 else cn[:, ch * 512 - 1:ch * 512]
            P.op("dve", lambda e, ch=ch, init=init: e.tensor_tensor_scan(
                cn[:, ch * 512:(ch + 1) * 512], ones16[:], nl[:, ch * 512:(ch + 1) * 512], init, ALU.mult, ALU.add),
                reads=[("nl", ch), "ones16", ("cn", ch - 1)], writes=[("cn", ch)])
        dq = K.sb("dq", [16, S], F32)
        hi = K.sb("hi", [16, S], BF16)
        lo = K.sb("lo", [16, S], BF16)
        onesb = K.sb("onesb", [16, S], BF16)
        P.op("pool", lambda e: e.memset(onesb[:], 1.0), writes=["onesb"])
        allcn = [("cn", ch) for ch in range(8)]
        for ch in range(8):
            sl = slice(ch * 512, (ch + 1) * 512)
            P.op("dve", lambda e, ch=ch, sl=sl: e.tensor_scalar(dq[:, sl], cn[:, sl], cn[:, ch * 512:ch * 512 + 1], -8.0,
                                                              ALU.subtract, ALU.mult),
                 reads=[("cn", ch)], writes=[("dq", ch)])
            P.op("dve", lambda e, sl=sl: e.tensor_copy(hi[:, sl], dq[:, sl]), reads=[("dq", ch)], writes=[("hi", ch)])
            P.op("dve", lambda e, sl=sl: e.tensor_tensor(lo[:, sl], dq[:, sl], hi[:, sl], ALU.subtract),
                 reads=[("dq", ch), ("hi", ch)], writes=[("lo", ch)])
        P.dma("sp", qaT[:, 64, :], hi[:], reads=[("hi", ch) for ch in range(8)], writes=["qa_hi"], stream="st")
        P.dma("sp", qaT[:, 65, :], lo[:], reads=[("lo", ch) for ch in range(8)], writes=["qa_lo"], stream="st")
        P.dma("sp", kaT[:, 64, :], onesb[:], reads=["onesb"], writes=["ka_1"], stream="st")
        P.dma("sp", kaT[:, 65, :], onesb[:], reads=["onesb"], writes=["ka_2"], stream="st")
        pct = pqk.t[0][:].rearrange("p (a b) -> p a b", b=16)
        pctk = ("pqk", 0)
        for j in range(32):
            P.op("pe", lambda e, j=j: e.transpose(pct[:, j, :], cn[:, j * 128:(j + 1) * 128], c["ident_f"][0:16, 0:16]),
                 reads=allcn + ["ident_f"], writes=[pctk])
        P.op("dve", lambda e: e.tensor_copy(ctok[:], pct), reads=[pctk], writes=["ctok"])
        sel0 = K.sb("sel0", [128, 128], F32)
        P.op("pool", lambda e: e.memset(sel0[:], 0.0), writes=["sel0"])
        P.op("pool", lambda e: e.memset(sel0[0:1, :], 1.0), writes=["sel0"])
        pcr = pqk.t[1][:, 0:128]
        pcrk = ("pqk", 1)
        crsrc = K.sb("crsrc", [128, 8, 16], F32)
        P.op("dve", lambda e: e.tensor_copy(crsrc[:], ctok[:, 0:32:4, :]), reads=["ctok"], writes=["crsrc"])
        P.op("pe", lambda e: e.matmul(pcr, sel0[:], crsrc[:].rearrange("p a b -> p (a b)"), start=True, stop=True),
             reads=["sel0", "crsrc"], writes=[pcrk])
        P.op("dve", lambda e: e.tensor_copy(cref[:].rearrange("p a b -> p (a b)"), pcr), reads=[pcrk], writes=["cref"])


def phase_attn(K, c, qaT, kaT, vaug, ctok, cref, attnT):
    P = K.P
    LOOK = 3
    with K.scope():
        qa = Pool(K, "qa", 2, [66, S], BF16)
        ka = Pool(K, "ka", 2, [66, S], BF16)
        va = Pool(K, "va", 2, [128, 32, 128], BF16)
        pS = Pool(K, "pS", 4, [128, 512], F32, psum=True)
        pO = Pool(K, "pO", 2, [128, 512], F32, psum=True)
        pT = Pool(K, "pT", 4, [128, 512], BF16)
        bias = Pool(K, "bias", 3, [128, 32], F32)
        rden = Pool(K, "rden", 2, [128, 512], F32)
        ost = Pool(K, "ost", 3, [64, 512], BF16)
        vview = vaug.rearrange("(j p) h d -> p j h d", p=128)
        heads = {}
        blocks = {}
        sbuf_s = {}

        def load_head(h):
            if h >= 16:
                return
            qt, qk = qa.next()
            kt, kk = ka.next()
            vt, vk = va.next()
            P.dma("sp", qt[:], qaT[h], reads=[("qk_d", True, h), "qa_hi", "qa_lo"], writes=[qk], stream="ld")
            P.dma("sp", kt[:], kaT[h], reads=[("qk_d", False, h), "ka_1", "ka_2"], writes=[kk], stream="ld")
            for jj in range(0, 32, 8):
                P.dma("sp", vt[:, jj:jj + 8, :], vview[:, jj:jj + 8, h, :],
                      reads=[("vaug_d", j) for j in range(jj, jj + 8)], writes=[(vk, jj)], stream="ld")
            heads[h] = (qt, qk, kt, kk, vt, vk)

        def geom(ch, j):
            i = j - 4 * ch
            q0 = 0 if i < 0 else 128 * i
            return i, q0, 512 - q0

        def emit_S(it):
            h, ch, j, nj = it
            qt, qk, kt, kk, vt, vk = heads[h]
            if j == 0:
                bt, bk = bias.next()
                P.op("dve", lambda e, bt=bt, nj=nj, h=h, ch=ch: e.tensor_scalar(
                    bt[:, 0:nj], ctok[:, 0:nj, h], cref[:, ch, h:h + 1], None, ALU.subtract),
                    reads=["ctok", "cref"], writes=[bk])
                po, pok = pO.next()
                blocks[(h, ch)] = (bt, bk, po, pok)
            i, q0, n = geom(ch, j)
            ps, psk = pS.next()
            P.op("pe", lambda e, ps=ps, kt=kt, qt=qt, j=j, q0=q0, n=n, ch=ch: e.matmul(
                ps[:, 0:n], kt[:, j * 128:(j + 1) * 128], qt[:, ch * 512 + q0:ch * 512 + 512],
                start=True, stop=True),
                reads=[qk, kk], writes=[psk])
            sbuf_s[it] = (ps, psk)

        def emit_rest(it):
            h, ch, j, nj = it
            qt, qk, kt, kk, vt, vk = heads[h]
            bt, bk, po, pok = blocks[(h, ch)]
            ps, psk = sbuf_s.pop(it)
            i, q0, n = geom(ch, j)
            pt, ptk = pT.next()
            P.op("act", lambda e, pt=pt, ps=ps, bt=bt, j=j, n=n: e.activation(
                pt[:, 0:n], ps[:, 0:n], AF.Exp, bias=bt[:, j:j + 1], scale=0.125),
                reads=[bk], writes=[ptk, psk])
            if i >= 0:
                P.op("pool", lambda e, pt=pt: e.affine_select(
                    pt[:, 0:128], pt[:, 0:128], [[1, 128]], ALU.is_ge, 0.0, base=0, channel_multiplier=-1),
                    reads=[ptk], writes=[ptk])
            P.op("pe", lambda e, po=po, vt=vt, pt=pt, j=j, q0=q0, n=n, nj=nj: e.matmul(
                po[:, q0:512], vt[:, j, :], pt[:, 0:n], start=(j == 0), stop=(j == nj - 1)),
                reads=[(vk, (j // 8) * 8), ptk], writes=[pok])
            if j == nj - 1:
                rd, rdk = rden.next()
                P.op("dve", lambda e, rd=rd, po=po: e.reciprocal(rd[64:128, :], po[64:128, :]), writes=[rdk, pok])
                os_, osk = ost.next()
                P.op("dve", lambda e, os_=os_, po=po, rd=rd: e.tensor_tensor(os_[:], po[0:64, :], rd[64:128, :], ALU.mult),
                     reads=[rdk], writes=[osk, pok])
                P.dma("sp", attnT[h * 64:(h + 1) * 64, ch * 512:(ch + 1) * 512], os_[:], reads=[osk],
                      writes=[("attn_d", ch)], stream="st")
                del blocks[(h, ch)]
                if ch == 7:
                    load_head(h + 2)

        items = [(h, ch, j, 4 * ch + 4) for h in range(16) for ch in range(8) for j in range(4 * ch + 4)]
        load_head(0)
        load_head(1)
        for k in range(LOOK):
            emit_S(items[k])
        for idx, it in enumerate(items):
            if idx + LOOK < len(items):
                emit_S(items[idx + LOOK])
            emit_rest(it)


def phase_proj_ln(K, c, inT, in_keys, kch, w_o, resid, resid_keys, g_row, b_row, h1, h1T, tag):
    P = K.P
    with K.scope():
        wo = load_w_bf16(K, "wo", w_o, kch, D)
        g_bc = load_bcast_row(K, "g_bc", g_row, D)
        b_bc = load_bcast_row(K, "b_bc", b_row, D)
        c["ln_stats"] = Pool(K, "lnst", 4, [128, 2, 6], F32)
        c["ln_mv"] = Pool(K, "lnmv", 4, [128, 8], F32)
        c["tp_b"] = Pool(K, "tpb", 2, [128, 8, 128], BF16, psum=True)
        c["tp_i"] = 0
        c["tp_eng"] = ("act", "dve")
        ain = Pool(K, "ain", 2, [128, kch, 512], BF16)
        xin = Pool(K, "rin", 4, [128, D], F32)
        pm = Pool(K, "pm", 3, [128, 2, 512], F32, psum=True)
        zt = Pool(K, "zt", 4, [128, D], F32)
        ht = Pool(K, "ht", 4, [128, D], F32)
        hb = Pool(K, "hb", 4, [128, D], BF16)
        hTs = Pool(K, "hTs", 2, [128, 8, 512], BF16)
        inv = inT.rearrange("(k p) t -> p k t", p=128)
        h1Tv = h1T.rearrange("(k p) t -> p k t", p=128)
        abuf = {}
        zs = {}
        stg = {}

        def mm(ti):
            ch, t = ti // 4, ti % 4
            tok0 = ti * 128
            if t == 0:
                a, ak = ain.next()
                for k0 in range(0, kch, 8):
                    P.dma("sp", a[:, k0:k0 + 8, :], inv[:, k0:k0 + 8, ch * 512:(ch + 1) * 512],
                          reads=in_keys(ch), writes=[(ak, k0)], stream="ld")
                abuf[ch] = (a, ak)
            a, ak = abuf[ch]
            xr, xrk = xin.next()
            P.dma("sp", xr[:], resid[tok0:tok0 + 128, :], reads=resid_keys(ti), writes=[xrk], stream="ld")
            pmt, pmk = pm.next()
            for half in range(2):
                for k in range(kch):
                    P.op("pe", lambda e, k=k, half=half: e.matmul(
                        pmt[:, half, :], a[:, k, t * 128:(t + 1) * 128], wo[:, k, half * 512:(half + 1) * 512],
                        start=(k == 0), stop=(k == kch - 1)),
                        reads=[(ak, (k // 8) * 8), ("wo", k)], writes=[(pmk, half)])
            z, zk = zt.next()
            P.op("dve", lambda e: e.scalar_tensor_tensor(
                z[:], xr[:], ALPHA, pmt[:].rearrange("p a b -> p (a b)"), ALU.mult, ALU.add),
                reads=[xrk], writes=[zk, (pmk, 0), (pmk, 1)])
            zs[ti] = (z, zk)

        def ln(ti):
            ch, t = ti // 4, ti % 4
            tok0 = ti * 128
            if t == 0:
                stg[ch] = hTs.next()
            hs, hsk = stg[ch]
            z, zk = zs.pop(ti)
            ho, hok = ht.next()
            ln_tile(K, c, z[:], zk, g_bc[:], b_bc[:], "g_bc", "b_bc", ho[:], hok)
            P.dma("pool", h1[tok0:tok0 + 128, :], ho[:], reads=[hok], writes=[(tag + "h1_d", ti)], stream="st")
            hbt, hbk = hb.next()
            P.op("act", lambda e: e.copy(hbt[:], ho[:]), reads=[hok], writes=[hbk])
            transpose_to_hT(K, c, hbt, hbk, hs, (hsk, t), 8, t * 128)

        for g0 in range(0, 32, 4):
            P.begin_group()
            for ti in range(g0, g0 + 4):
                P.next_stream()
                mm(ti)
                ln(ti)
            P.end_group()
            ch = g0 // 4
            hs, hsk = stg.pop(ch)
            P.dma("pool", h1Tv[:, :, ch * 512:(ch + 1) * 512], hs[:], reads=[(hsk, t) for t in range(4)],
                  writes=[(tag + "h1T_d", ch)], stream="st")


def epilogue_s1(K, c, z, zk, tok0, g_bc, b_bc, p_d):
    P = K.P
    h2, h2k = c["h2"].next()
    ln_tile(K, c, z[:], zk, g_bc[:], b_bc[:], "g2_bc", "b2_bc", h2[:], h2k)
    hbt, hbk = c["hb2"].next()
    P.op("act", lambda e: e.copy(hbt[:], h2[:]), reads=[h2k], writes=[hbk])
    h2T, h2Tk = c["h2T"].next()
    transpose_to_hT(K, c, hbt, hbk, h2T, h2Tk, 8, 0)
    pt_, ptk = c["pin"].next()
    P.dma("sp", pt_[:], p_d[tok0:tok0 + 128, :], writes=[ptk], stream="ld")
    pb, pbk = c["pb"].next()
    P.op("dve", lambda e: e.tensor_copy(pb[:], pt_[:]), reads=[ptk], writes=[pbk])
    pT, pTk = c["pT"].next()
    transpose_to_hT(K, c, pb, pbk, pT, pTk, 2, 0)
    return (h2, h2k, h2T, h2Tk, pT, pTk, tok0)


def epilogue_s2(K, c, ctx, wpg, wpp, out_d, out_key, outT_stage):
    P = K.P
    h2, h2k, h2T, h2Tk, pT, pTk, tok0 = ctx
    pgp, pgpk = c["pg"].next()
    sg, sgk = c["sg"].next()
    for half in range(2):
        cs = slice(half * 512, (half + 1) * 512)
        for k in range(8):
            P.op("pe", lambda e, k=k, cs=cs: e.matmul(pgp[:, 0, :], h2T[:, k, :], wpg[:, k, cs], start=(k == 0), stop=(k == 7)),
                 reads=[h2Tk, ("wpg", k)], writes=[(pgpk, 0)])
        for k in range(2):
            P.op("pe", lambda e, k=k, cs=cs: e.matmul(pgp[:, 1, :], pT[:, k, :], wpp[:, k, cs], start=(k == 0), stop=(k == 1)),
                 reads=[pTk, ("wpp", k)], writes=[(pgpk, 1)])
        P.op("act", lambda e, cs=cs: e.activation(sg[:, cs], pgp[:, 0, :], AF.Sigmoid), writes=[(sgk, half), (pgpk, 0)])
        P.op("dve", lambda e, cs=cs: e.tensor_tensor(sg[:, cs], pgp[:, 1, :], sg[:, cs], ALU.mult),
             reads=[(sgk, half)], writes=[(sgk, half), (pgpk, 1)])
    h3, h3k = c["h3"].next()
    P.op("pool", lambda e: e.tensor_tensor(h3[:], h2[:], sg[:], ALU.add), reads=[h2k, (sgk, 0), (sgk, 1)], writes=[h3k])
    P.dma("pool", out_d[tok0:tok0 + 128, :], h3[:], reads=[h3k], writes=[(out_key, tok0 // 128)], stream="st")
    if outT_stage is not None:
        hs, hsk, toff = outT_stage
        hb3, hb3k = c["hb2"].next()
        P.op("act", lambda e: e.copy(hb3[:], h3[:]), reads=[h3k], writes=[hb3k])
        transpose_to_hT(K, c, hb3, hb3k, hs, hsk, 8, toff)


def epilogue_setup(K, c, g_row, b_row, w_pg, w_pp, npg=1, nb=2):
    g_bc = load_bcast_row(K, "g2_bc", g_row, D)
    b_bc = load_bcast_row(K, "b2_bc", b_row, D)
    wpg = load_w_bf16(K, "wpg", w_pg, 8, D)
    wpp = load_w_bf16(K, "wpp", w_pp, 2, D)
    c["ln_stats"] = Pool(K, "lnst", nb, [128, 2, 6], F32)
    c["ln_mv"] = Pool(K, "lnmv", nb, [128, 8], F32)
    c["tp_b"] = Pool(K, "tpb", 2, [128, 8, 128], BF16, psum=True)
    c["tp_i"] = 0
    c["tp_eng"] = ("act", "dve")
    c["h2"] = Pool(K, "h2", nb, [128, D], F32)
    c["hb2"] = Pool(K, "hb2", nb, [128, D], BF16)
    c["h2T"] = Pool(K, "h2T", nb, [128, 8, 128], BF16)
    c["pin"] = Pool(K, "pin", nb, [128, 256], F32)
    c["pb"] = Pool(K, "pb", nb, [128, 256], BF16)
    c["pT"] = Pool(K, "pT", nb, [128, 2, 128], BF16)
    c["pg"] = Pool(K, "pg", npg, [128, 2, 512], F32, psum=True)
    c["sg"] = Pool(K, "sg", max(1, nb // 2), [128, D], F32)
    c["h3"] = Pool(K, "h3", nb, [128, D], F32)
    return g_bc, b_bc, wpg, wpp


def phase_ffn(K, c, h1, h1T, w_g, w_u, w_d, g_row, b_row, w_pg, w_pp, p_d, out_d, outT_d, actT_d):
    P = K.P
    NJ = FFN // 128
    h1Tv = h1T.rearrange("(k p) t -> p k t", p=128)
    with K.scope():
        hres = K.sb("hres", [128, 8, S], BF16)
        for ch in range(8):
            P.dma("sp", hres[:, :, ch * 512:(ch + 1) * 512], h1Tv[:, :, ch * 512:(ch + 1) * 512],
                  reads=[("l0h1T_d", ch)], writes=[("hres", ch)], stream="ld")
        wgu = Pool(K, "wgu", 3, [128, 2, 8, 256], BF16)
        pgu = Pool(K, "pgu", 3, [128, 2, 512], F32, psum=True)
        sgt = Pool(K, "sgt", 3, [128, 512], F32)
        ast = Pool(K, "ast", 3, [128, S], BF16)
        wgr = w_g.rearrange("(k p) c -> p k c", p=128)
        wur = w_u.rearrange("(k p) c -> p k c", p=128)
        for j2 in range(NJ // 2):
            wt, wtk = wgu.next()
            P.dma("pool", wt[:, 0], wgr[:, :, j2 * 256:(j2 + 1) * 256], writes=[(wtk, 0)], stream="wc")
            P.dma("pool", wt[:, 1], wur[:, :, j2 * 256:(j2 + 1) * 256], writes=[(wtk, 1)], stream="wc")
            for jj in range(2):
                j = j2 * 2 + jj
                a_, ak = ast.next()
                for blk in range(8):
                    bs = slice(blk * 512, (blk + 1) * 512)
                    pg_, pgk_ = pgu.next()
                    for gu in range(2):
                        for k in range(8):
                            P.op("pe", lambda e, pg_=pg_, wt=wt, gu=gu, k=k, jj=jj, bs=bs: e.matmul(
                                pg_[:, gu, :], wt[:, gu, k, jj * 128:(jj + 1) * 128], hres[:, k, bs],
                                start=(k == 0), stop=(k == 7)),
                                reads=[(wtk, gu), ("hres", blk)], writes=[(pgk_, gu)])
                    sg_, sgk_ = sgt.next()
                    P.op("act", lambda e, sg_=sg_, pg_=pg_: e.activation(sg_[:], pg_[:, 0, :], AF.Silu),
                         writes=[sgk_, (pgk_, 0)])
                    P.op("dve", lambda e, a_=a_, bs=bs, sg_=sg_, pg_=pg_: e.tensor_tensor(a_[:, bs], sg_[:], pg_[:, 1, :], ALU.mult),
                         reads=[sgk_], writes=[(ak, blk), (pgk_, 1)])
                P.dma("sp", actT_d[j * 128:(j + 1) * 128, :], a_[:], reads=[(ak, b) for b in range(8)],
                      writes=[("actT_d", j)], stream="st")
    with K.scope():
        g_bc, b_bc, wpg, wpp = epilogue_setup(K, c, g_row, b_row, w_pg, w_pp, npg=1, nb=3)
        wd = load_w_bf16(K, "wd", w_d, NJ, D)
        ain = Pool(K, "fain", 4, [128, NJ, 128], BF16)
        pf_ = Pool(K, "pf", 2, [128, 2, 512], F32, psum=True)
        h1in = Pool(K, "h1in", 4, [128, D], F32)
        zt = Pool(K, "zt", 4, [128, D], F32)
        hTs = Pool(K, "hTs", 2, [128, 8, 512], BF16)
        actv = actT_d.rearrange("(j p) t -> p j t", p=128)
        outTv = outT_d.rearrange("(k p) t -> p k t", p=128) if outT_d is not None else None

        def down(ti):
            a, ak = ain.next()
            for j0 in range(0, NJ, 11):
                P.dma("sp", a[:, j0:j0 + 11, :], actv[:, j0:j0 + 11, ti * 128:(ti + 1) * 128],
                      reads=[("actT_d", j) for j in range(j0, j0 + 11)], writes=[(ak, j0)], stream="ld")
            h1t, h1k = h1in.next()
            P.dma("sp", h1t[:], h1[ti * 128:(ti + 1) * 128, :], reads=[("l0h1_d", ti)], writes=[h1k], stream="ld")
            pf, pfk = pf_.next()
            for half in range(2):
                for j in range(NJ):
                    P.op("pe", lambda e, j=j, half=half: e.matmul(
                        pf[:, half, :], a[:, j, :], wd[:, j, half * 512:(half + 1) * 512],
                        start=(j == 0), stop=(j == NJ - 1)),
                        reads=[(ak, (j // 11) * 11), ("wd", j)], writes=[(pfk, half)])
            z, zk = zt.next()
            P.op("dve", lambda e: e.scalar_tensor_tensor(
                z[:], h1t[:], ALPHA, pf[:].rearrange("p a b -> p (a b)"), ALU.mult, ALU.add),
                reads=[h1k], writes=[zk, (pfk, 0), (pfk, 1)])
            return z, zk

        for blk in range(8):
            hs, hsk = hTs.next() if outTv is not None else (None, None)
            P.begin_group()
            for t in range(4):
                ti = blk * 4 + t
                P.next_stream()
                z, zk = down(ti)
                ctx = epilogue_s1(K, c, z, zk, ti * 128, g_bc, b_bc, p_d)
                epilogue_s2(K, c, ctx, wpg, wpp, out_d, "out_d", (hs, (hsk, t), t * 128) if outTv is not None else None)
            P.end_group()
            if outTv is not None:
                P.dma("pool", outTv[:, :, blk * 512:(blk + 1) * 512], hs[:], reads=[(hsk, t) for t in range(4)],
                      writes=[("outT_d", blk)], stream="st")


def phase_ssd_proj(K, c, hT_d, hT_key, w_in, conv_w, conv_b, dt_bias, xs_d, Btok_d, BT_d, CT_d, dt_tok):
    P = K.P
    with K.scope():
        wx = load_w_bf16(K, "wx", w_in, 8, 4096, 2048)
        wdt = load_w_bf16(K, "wdt", w_in, 8, 32, 6144)
        dtb = load_bcast_row(K, "dtb", dt_bias, 32)
        pxb = Pool(K, "pxb", 4, [128, 512], F32, psum=True)
        tpf = Pool(K, "tpf", 2, [128, 4, 128], F32, psum=True)
        tpb = Pool(K, "tpb", 1, [128, 8, 128], BF16, psum=True)
        pdt = K.ps("pdt", [128, 512], F32)
        cwb = K.sb("cwb", [128, 32, 5], F32)
        with K.scope():
            cw5 = K.sb("cw5", [5, 4096], F32)
            P.dma("sp", cw5[0:4, :], conv_w, writes=["cw5a"], stream="cst")
            P.dma("sp", cw5[4:5, :], conv_b.rearrange("(o c) -> o c", o=1), writes=["cw5b"], stream="cst")
            pcw = pxb.t[0][:, 0:160].rearrange("p (a b) -> p a b", b=5)
            for i in range(32):
                P.op("pe", lambda e, i=i: e.transpose(pcw[:, i, :], cw5[:, i * 128:(i + 1) * 128], c["ident_f"][0:5, 0:5]),
                     reads=["cw5a", "cw5b", "ident_f"], writes=[("pxb", 0)])
            P.op("dve", lambda e: e.tensor_copy(cwb[:], pcw), writes=["cwb", ("pxb", 0)])
        carry = K.sb("carry", [128, 32, 3], F32)
        P.op("pool", lambda e: e.memset(carry[:], 0.0), writes=[("carry", i) for i in range(32)])
        hin = Pool(K, "hin", 2, [128, 8, 512], BF16)
        pre = Pool(K, "pre", 8, [128, 515], F32)
        acc = Pool(K, "acc", 8, [128, 512], F32)
        sil = Pool(K, "sil", 12, [128, 512], F32)
        silb = Pool(K, "silb", 12, [128, 512], BF16)
        xst = Pool(K, "xst", 4, [128, 2048], F32)
        bst = Pool(K, "bst", 4, [128, 1024], BF16)
        e1 = Pool(K, "e1", 2, [128, 128], F32)
        hTv = hT_d.rearrange("(k p) t -> p k t", p=128)
        for ch in range(8):
            hi_, hik = hin.next()
            P.dma("sp", hi_[:], hTv[:, :, ch * 512:(ch + 1) * 512], reads=hT_key(ch), writes=[hik], stream="ld")
            for t in range(4):
                for k in range(8):
                    P.op("pe", lambda e, k=k, t=t, hi_=hi_: e.matmul(pdt[:, t * 32:(t + 1) * 32], hi_[:, k, t * 128:(t + 1) * 128],
                                                                    wdt[:, k, :], start=(k == 0), stop=(k == 7)),
                         reads=[hik, ("wdt", k)], writes=["pdt"])
            ee, eek = e1.next()
            P.op("dve", lambda e, ee=ee: e.tensor_tensor(ee[:].rearrange("p (t r) -> p t r", t=4),
                                                         pdt[:, 0:128].rearrange("p (t r) -> p t r", t=4), bc_mid(dtb[:], 4), ALU.add),
                 reads=["dtb"], writes=[eek, "pdt"])
            P.op("act", lambda e, ee=ee: e.activation(ee[:], ee[:], AF.Exp), reads=[eek], writes=[eek])
            P.op("act", lambda e, ee=ee, ch=ch: e.activation(dt_tok[:, ch * 4:(ch + 1) * 4, :].rearrange("p t r -> p (t r)"), ee[:],
                                                             AF.Ln, bias=1.0, scale=1.0),
                 reads=[eek], writes=[("dt_tok", ch * 4 + t) for t in range(4)])
            xs_t = [xst.next() for _ in range(4)]
            bs_t = [bst.next() for _ in range(4)]
            def do_tr(i4, outs):
                if i4 < 4:
                    for t in range(4):
                        pt, ptk = tpf.next()
                        for ii in range(4):
                            so, sok = outs[ii]
                            P.op("pe", lambda e, pt=pt, ii=ii, so=so, t=t: e.transpose(
                                pt[:, ii, :], so[:, t * 128:(t + 1) * 128], c["ident_f"][:]),
                                reads=[sok, "ident_f"], writes=[ptk])
                        xs_, xsk = xs_t[t]
                        if t % 2 == 0:
                            P.op("act", lambda e, xs_=xs_, pt=pt, i4=i4: e.copy(
                                xs_[:, i4 * 512:(i4 + 1) * 512], pt[:].rearrange("p a b -> p (a b)")),
                                reads=[ptk], writes=[(xsk, i4)])
                        else:
                            P.op("dve", lambda e, xs_=xs_, pt=pt, i4=i4: e.tensor_copy(
                                xs_[:, i4 * 512:(i4 + 1) * 512], pt[:].rearrange("p a b -> p (a b)")),
                                reads=[ptk], writes=[(xsk, i4)])
                elif i4 < 6:
                    for t in range(4):
                        pt, ptk = tpb.next()
                        for ii in range(4):
                            so, sok = outs[ii]
                            P.op("pe", lambda e, pt=pt, ii=ii, so=so, t=t: e.transpose(
                                pt[:, ii, :], so[:, t * 128:(t + 1) * 128], c["ident_b"][:]),
                                reads=[sok, "ident_b"], writes=[ptk])
                        bs_, bsk = bs_t[t]
                        gi = i4 - 4
                        P.op("dve", lambda e, bs_=bs_, pt=pt, gi=gi: e.tensor_copy(
                            bs_[:, gi * 512:(gi + 1) * 512], pt[:, 0:4, :].rearrange("p a b -> p (a b)")),
                            reads=[ptk], writes=[(bsk, gi)])

            actx = {}
            bouts = {}

            def stageA(i4):
                lst = []
                for ii in range(4):
                    P.next_stream()
                    i = i4 * 4 + ii
                    px, pxk = pxb.next()
                    for k in range(8):
                        P.op("pe", lambda e, k=k, i=i, px=px, hi_=hi_: e.matmul(
                            px[:], wx[:, k, i * 128:(i + 1) * 128], hi_[:, k, :], start=(k == 0), stop=(k == 7)),
                            reads=[hik, ("wx", k)], writes=[pxk])
                    pr, prk = pre.next()
                    P.op("act", lambda e, pr=pr, px=px: e.copy(pr[:, 3:515], px[:]), writes=[(prk, 1), pxk])
                    P.op("pool", lambda e, pr=pr, i=i: e.tensor_copy(pr[:, 0:3], carry[:, i, :]), reads=[("carry", i)], writes=[(prk, 0)])
                    ac, ack = acc.next()
                    P.op("pool", lambda e, ac=ac, pr=pr, i=i: e.tensor_scalar(
                        ac[:], pr[:, 3:515], cwb[:, i, 3:4], cwb[:, i, 4:5], ALU.mult, ALU.add),
                        reads=[(prk, 1), "cwb"], writes=[ack])
                    lst.append((i, pr, prk, ac, ack))
                actx[i4] = lst

            def stageB(i4):
                outs = []
                for (i, pr, prk, ac, ack) in actx.pop(i4):
                    P.next_stream()
                    for kk in (2, 1, 0):
                        P.op("dve", lambda e, ac=ac, pr=pr, i=i, kk=kk: e.scalar_tensor_tensor(
                            ac[:], pr[:, kk:kk + 512], cwb[:, i, kk:kk + 1], ac[:], ALU.mult, ALU.add),
                            reads=[(prk, 0), (prk, 1), "cwb", ack], writes=[ack])
                    P.op("dve", lambda e, pr=pr, i=i: e.tensor_copy(carry[:, i, :], pr[:, 512:515]),
                         reads=[(prk, 1)], writes=[("carry", i)])
                    if i < 16:
                        so, sok = sil.next()
                    else:
                        so, sok = silb.next()
                    P.op("act", lambda e, so=so, ac=ac: e.activation(so[:], ac[:], AF.Silu), reads=[ack], writes=[sok])
                    outs.append((so, sok))
                    if i >= 16:
                        g = (i - 16) % 8
                        dst = BT_d if i < 24 else CT_d
                        P.dma("act", dst[g, :, ch * 512:(ch + 1) * 512], so[:], reads=[sok],
                              writes=[("BC_d", i < 24, g, ch)], stream="st")
                bouts[i4] = outs

            for step in range(10):
                if step <= 8:
                    P.begin_group()
                    if step < 8:
                        stageA(step)
                    if step >= 1:
                        stageB(step - 1)
                    P.end_group()
                if step >= 2:
                    do_tr(step - 2, bouts.pop(step - 2))
            for t in range(4):
                ti = ch * 4 + t
                xs_, xsk = xs_t[t]
                bs_, bsk = bs_t[t]
                P.dma("pool", xs_d[ti * 128:(ti + 1) * 128, :], xs_[:], reads=[(xsk, q) for q in range(4)],
                      writes=[("xs_d", ti)], stream="st")
                P.dma("pool", Btok_d[ti * 128:(ti + 1) * 128, :], bs_[:], reads=[(bsk, q) for q in range(2)],
                      writes=[("Btok_d", ti)], stream="st")


def bc_mid(ap2d, n):
    sh = list(ap2d.shape)
    return ap2d.unsqueeze(1).broadcast_to([sh[0], n, sh[1]])


def bc_last(ap2d, n):
    sh = list(ap2d.shape)
    return ap2d.unsqueeze(2).broadcast_to([sh[0], sh[1], n])


def phase_ssd_core(K, c, xs_d, Btok_d, BT_d, CT_d, dt_tok, a_log, d_skip, y_d):
    P = K.P
    with K.scope():
        tri = K.sb("tri", [128, 128], F32)
        P.op("pool", lambda e: e.memset(tri[:], 1.0), writes=["tri"])
        P.op("pool", lambda e: e.affine_select(tri[:], tri[:], [[1, 128]], ALU.is_ge, 0.0, base=0, channel_multiplier=-1),
             reads=["tri"], writes=["tri"])
        sel127 = K.sb("sel127", [128, 128], F32)
        P.op("pool", lambda e: e.memset(sel127[:], 1.0), writes=["sel127"])
        P.op("pool", lambda e: e.affine_select(sel127[:], sel127[:], [[0, 128]], ALU.is_equal, 0.0, base=-127, channel_multiplier=1),
             reads=["sel127"], writes=["sel127"])
        ind = K.sb("ind", [96, 32, 128], BF16)
        P.op("pool", lambda e: e.memset(ind[:], 1.0), writes=["ind"])
        for q in range(3):
            P.op("pool", lambda e, q=q: e.affine_select(ind[32 * q:32 * q + 32], ind[32 * q:32 * q + 32], [[-1, 32], [0, 128]],
                                                        ALU.is_equal, 0.0, base=0, channel_multiplier=1),
                 reads=["ind"], writes=["ind"])
        a_bc = load_bcast_row(K, "a_bc", a_log, 32)
        P.op("act", lambda e: e.activation(a_bc[:], a_bc[:], AF.Exp), reads=["a_bc"], writes=["a_bc"])
        P.op("dve", lambda e: e.tensor_scalar(a_bc[:], a_bc[:], -1.0, None, ALU.mult), reads=["a_bc"], writes=["a_bc"])
        dsk = load_bcast_row(K, "dsk", d_skip, 32)
        hT = K.sb("hT", [128, 2048], F32)
        hTb = K.sb("hTb", [128, 2048], BF16)
        P.op("pool", lambda e: e.memset(hT[:], 0.0), writes=["hT"])
        P.op("pool", lambda e: e.memset(hTb[:], 0.0), writes=["hTb"])
        xsc = Pool(K, "xsc", 3, [128, 2048], F32)
        btc = Pool(K, "btc", 3, [128, 1024], BF16)
        BTc = Pool(K, "BTc", 3, [128, 8, 128], BF16)
        CTc = Pool(K, "CTc", 3, [128, 8, 128], BF16)
        sm = Pool(K, "sm", 3, [128, 8, 32], F32)
        dap = Pool(K, "dap", 3, [128, 128], F32)
        for j in range(3):
            P.op("pool", lambda e, j=j: e.memset(dap.t[j][:], 0.0), writes=[("dap", j)])
        dT = Pool(K, "dT", 3, [96, 2, 128], BF16)
        dsc = Pool(K, "dsc", 3, [96, 4, 128], F32)
        dec = Pool(K, "dec", 4, [128, 4, 128], F32)
        MT = Pool(K, "MT", 16, [128, 4, 128], BF16)
        xdt = Pool(K, "xdt", 3, [128, 32, 64], BF16)
        xdte = Pool(K, "xdte", 3, [128, 32, 64], BF16)
        ydg = Pool(K, "ydg", 1, [128, 2048], F32)
        yraw = Pool(K, "yraw", 1, [128, 8, 512], F32)
        stT = K.sb("stT", [128, 2048], F32)
        yo = Pool(K, "yo", 2, [128, 2048], F32)
        pmisc = K.ps("pmisc", [128, 512], F32)
        pcbp = Pool(K, "pcb", 1, [128, 512], F32, psum=True)
        cbs = Pool(K, "cbs", 2, [128, 2, 512], F32)
        pD = Pool(K, "pD", 2, [128, 512], F32, psum=True)
        pY = Pool(K, "pY", 2, [128, 512], F32, psum=True)
        pStp = Pool(K, "pSt", 2, [128, 512], F32, psum=True)
        BTv = BT_d.rearrange("g n t -> n g t")
        CTv = CT_d.rearrange("g n t -> n g t")
        st = {}

        sta = {}

        def pre_a(ch):
            P.next_stream()
            ts = slice(ch * 128, (ch + 1) * 128)
            xs_, xsk = xsc.next()
            bt_, btk = btc.next()
            BT_, BTk = BTc.next()
            CT_, CTk = CTc.next()
            P.dma("sp", xs_[:], xs_d[ts, :], reads=[("xs_d", ch)], writes=[xsk], stream="ld")
            P.dma("sp", bt_[:], Btok_d[ts, :], reads=[("Btok_d", ch)], writes=[btk], stream="ld")
            P.dma("sp", BT_[:], BTv[:, :, ts], reads=[("BC_d", True, g, ch // 4) for g in range(8)], writes=[BTk], stream="ld")
            P.dma("sp", CT_[:], CTv[:, :, ts], reads=[("BC_d", False, g, ch // 4) for g in range(8)], writes=[CTk], stream="ld")
            s_, sk = sm.next()
            dtc = dt_tok[:, ch, :]
            _, dacs, wdte, el, cdb, lastb, tmp = (s_[:, q, :] for q in range(7))
            dp_, dpk = dap.next()
            da = dp_[:, 0:32]
            P.op("dve", lambda e: e.tensor_tensor(dp_[:, 0:96].rearrange("p (q r) -> p q r", q=3), bc_mid(dtc, 3),
                                                  bc_mid(a_bc[:], 3), ALU.mult),
                 reads=[("dt_tok", ch), "a_bc"], writes=[dpk])
            P.op("pe", lambda e: e.matmul(pmisc[:, 0:32], tri[:], da, start=True, stop=True), reads=["tri", dpk], writes=["pmisc"])
            P.op("act", lambda e: e.copy(dacs, pmisc[:, 0:32]), writes=["pmisc", (sk, "dacs")])
            P.op("pe", lambda e: e.matmul(pmisc[:, 32:64], sel127[:], dacs, start=True, stop=True),
                 reads=["sel127", (sk, "dacs")], writes=["pmisc"])
            P.op("pe", lambda e: e.matmul(pmisc[:, 64:192], dp_[:], tri[:], start=True, stop=True), reads=["tri", dpk], writes=["pmisc"])
            d_, dk = dT.next()
            w_, wk = dsc.next()
            src = pmisc[0:96, 64:192]
            hb_, r1, mb_, r2 = (w_[:, q, :] for q in range(4))
            P.op("act", lambda e: e.copy(d_[:, 0, :], src), writes=["pmisc", (dk, "h")])
            P.op("dve", lambda e: e.tensor_tensor(r1, src, d_[:, 0, :], ALU.subtract), reads=[(dk, "h")], writes=["pmisc", (wk, 1)])
            P.op("act", lambda e: e.copy(d_[:, 1, :], r1), reads=[(wk, 1)], writes=[(dk, "m")])
            P.op("dve", lambda e: e.tensor_tensor(r2[64:96], r1[64:96], d_[64:96, 1, :], ALU.subtract),
                 reads=[(wk, 1), (dk, "m")], writes=[(wk, 3)])
            P.op("act", lambda e: e.copy(d_[32:64, 0, :], d_[32:64, 1, :]), reads=[(dk, "m"), (dk, "h")], writes=[(dk, "s1")])
            P.op("dve", lambda e: e.tensor_copy(d_[64:96, 0, :], r2[64:96]), reads=[(wk, 3), (dk, "h")], writes=[(dk, "s2")])
            P.op("dve", lambda e: e.tensor_scalar(d_[:, 1, :], d_[:, 0, :], -1.0, None, ALU.mult),
                 reads=[(dk, "s1"), (dk, "s2"), (dk, "h"), (wk, 3)], writes=[(dk, 1), (dk, "m")])
            P.op("act", lambda e: e.copy(lastb, pmisc[:, 32:64]), writes=["pmisc", (sk, "last")])
            P.op("act", lambda e: e.activation(cdb, pmisc[:, 32:64], AF.Exp), writes=["pmisc", (sk, "cdb")])
            P.op("act", lambda e: e.activation(el, dacs, AF.Exp), reads=[(sk, "dacs")], writes=[(sk, "el")])
            P.op("dve", lambda e: e.tensor_tensor(tmp, lastb, dacs, ALU.subtract), reads=[(sk, "last"), (sk, "dacs")], writes=[(sk, "tmp")])
            P.op("act", lambda e: e.activation(tmp, tmp, AF.Exp), reads=[(sk, "tmp")], writes=[(sk, "tmp")])
            P.op("dve", lambda e: e.tensor_tensor(wdte, tmp, dtc, ALU.mult), reads=[(sk, "tmp"), ("dt_tok", ch)], writes=[(sk, "wdte")])
            xd, xdk = xdt.next()
            xe, xek = xdte.next()
            xs3 = xs_[:].rearrange("p (r d) -> p r d", d=64)
            P.op("dve", lambda e: e.tensor_tensor(xd[:], xs3, bc_last(dtc, 64), ALU.mult), reads=[xsk, ("dt_tok", ch)], writes=[xdk])
            P.op("pool", lambda e: e.tensor_tensor(xe[:], xs3, bc_last(wdte, 64), ALU.mult), reads=[xsk, (sk, "wdte")], writes=[xek])
            sta[ch] = (xs_, xsk, xs3, bt_, btk, BT_, BTk, CT_, CTk, sk, el, cdb, xd, xdk, xe, xek, d_, dk)

        def pre_b(ch):
            xs_, xsk, xs3, bt_, btk, BT_, BTk, CT_, CTk, sk, el, cdb, xd, xdk, xe, xek, d_, dk = sta.pop(ch)
            mts = []
            P.next_stream()
            cb_, cbk = cbs.next()
            pcb, pcbk = pcbp.next()
            for half in range(2):
                for q in range(4):
                    P.op("pe", lambda e, half=half, q=q: e.matmul(pcb[:, q * 128:(q + 1) * 128], BT_[:, half * 4 + q, :],
                                                                  CT_[:, half * 4 + q, :], start=True, stop=True),
                         reads=[BTk, CTk], writes=[pcbk])
                P.op("act", lambda e, half=half: e.copy(cb_[:, half, :], pcb[:]), writes=[pcbk, (cbk, half)])
            for g in range(8):
                P.next_stream()
                pd_, pdk = pD.next()
                P.op("pe", lambda e, pd_=pd_, g=g: e.matmul(
                    pd_[:], d_[:, 1, :], ind[:, 4 * g:4 * g + 4, :].rearrange("p a b -> p (a b)"), start=True, stop=False),
                    reads=["ind", (dk, 1)], writes=[pdk])
                for r in range(4):
                    P.op("pe", lambda e, pd_=pd_, g=g, r=r: e.matmul(
                        pd_[:, r * 128:(r + 1) * 128], ind[:, 4 * g + r, :], d_[:, 0, :], start=False, stop=(r == 3)),
                        reads=["ind", (dk, 1), (dk, "s1"), (dk, "s2"), (dk, "h")], writes=[pdk])
                de, dek = dec.next()
                P.op("act", lambda e, de=de, pd_=pd_: e.activation(de[:].rearrange("p a b -> p (a b)"), pd_[:], AF.Exp),
                     writes=[dek, pdk])
                P.op("pool", lambda e, de=de: e.affine_select(de[:], de[:], [[0, 4], [1, 128]], ALU.is_ge, 0.0, base=0, channel_multiplier=-1),
                     reads=[dek], writes=[dek])
                mt, mtk = MT.next()
                P.op("dve", lambda e, mt=mt, de=de, g=g: e.tensor_tensor(
                    mt[:], de[:], bc_mid(cb_[:, g // 4, (g % 4) * 128:(g % 4) * 128 + 128], 4), ALU.mult),
                     reads=[dek, (cbk, g // 4)], writes=[mtk])
                mts.append((mt, mtk))
            st[ch] = (xs_, xsk, xs3, bt_, btk, CT_, CTk, sk, el, cdb, xd, xdk, xe, xek, mts)

        def main(ch):
            ts = slice(ch * 128, (ch + 1) * 128)
            xs_, xsk, xs3, bt_, btk, CT_, CTk, sk, el, cdb, xd, xdk, xe, xek, mts = st.pop(ch)
            yr, yrk = yraw.next()
            for g in range(8):
                P.next_stream()
                gs = slice(g * 256, (g + 1) * 256)
                mt, mtk = mts[g]
                py, pyk = pY.next()
                for r in range(4):
                    P.op("pe", lambda e, py=py, mt=mt, g=g, r=r: e.matmul(
                        py[:, r * 64:(r + 1) * 64], mt[:, r, :], xd[:, 4 * g + r, :], start=True, stop=True),
                        reads=[mtk, xdk], writes=[pyk])
                P.op("pe", lambda e, py=py, g=g, gs=gs: e.matmul(py[:, 256:512], CT_[:, g, :], hTb[:, gs], start=True, stop=True),
                     reads=[CTk, "hTb"], writes=[pyk])
                ps_, psk_ = pStp.next()
                P.op("pe", lambda e, g=g, ps_=ps_: e.matmul(
                    ps_[:, 0:256], bt_[:, g * 128:(g + 1) * 128], xe[:, 4 * g:4 * g + 4, :].rearrange("p a b -> p (a b)"),
                    start=True, stop=True),
                    reads=[btk, xek], writes=[psk_])
                P.op("act", lambda e, py=py, g=g: e.copy(yr[:, g, :], py[:]), writes=[pyk, (yrk, g)])
                P.op("act", lambda e, gs=gs, ps_=ps_: e.copy(stT[:, gs], ps_[:, 0:256]), writes=[psk_, ("stT", g)])
            P.next_stream()
            P.op("dve", lambda e: e.tensor_tensor(hT[:].rearrange("p (r d) -> p r d", d=64), hT[:].rearrange("p (r d) -> p r d", d=64),
                                                  bc_last(cdb, 64), ALU.mult),
                 reads=["hT", (sk, "cdb")], writes=["hT"])
            P.op("dve", lambda e: e.tensor_tensor(hT[:], hT[:], stT[:], ALU.add),
                 reads=["hT"] + [("stT", g) for g in range(8)], writes=["hT"])
            P.op("act", lambda e: e.copy(hTb[:], hT[:]), reads=["hT"], writes=["hTb"])
            P.next_stream()
            y_, yk = yo.next()
            yd, ydk = ydg.next()
            yrall = [(yrk, g) for g in range(8)]
            P.op("dve", lambda e: e.tensor_tensor(
                y_[:].rearrange("p (g r d) -> p g r d", g=8, r=4), yr[:, :, 256:512].rearrange("p g (r d) -> p g r d", d=64),
                el.rearrange("p (g r) -> p g r", r=4).unsqueeze(3).broadcast_to([128, 8, 4, 64]), ALU.mult),
                reads=yrall + [(sk, "el")], writes=[yk])
            P.op("pool", lambda e: e.tensor_tensor(y_[:].rearrange("p (g c) -> p g c", g=8), y_[:].rearrange("p (g c) -> p g c", g=8),
                                                   yr[:, :, 0:256], ALU.add),
                 reads=yrall + [yk], writes=[yk])
            P.op("dve", lambda e: e.tensor_tensor(yd[:].rearrange("p (r d) -> p r d", d=64), xs3, bc_last(dsk[:], 64), ALU.mult),
                 reads=[xsk, "dsk"], writes=[ydk])
            P.op("pool", lambda e: e.tensor_tensor(y_[:], y_[:], yd[:], ALU.add), reads=[yk, ydk], writes=[yk])
            P.dma("pool", y_d[ts, :], y_[:], reads=[yk], writes=[("y_d", ch)], stream="st")

        P.begin_group()
        pre_a(0)
        P.end_group()
        P.begin_group()
        pre_b(0)
        pre_a(1)
        P.end_group()
        for ch in range(32):
            P.begin_group()
            main(ch)
            if ch + 1 < 32:
                pre_b(ch + 1)
            if ch + 2 < 32:
                pre_a(ch + 2)
            P.end_group()


def phase_ssd_gate(K, c, y_d, hT_d, hT_key, w_in, norm_g, ynT_d):
    P = K.P
    with K.scope():
        wz = load_w_bf16(K, "wz", w_in, 8, 2048, 0)
        ng = load_bcast_row(K, "ng", norm_g, 2048)
        eps_r = K.sb("eps_r", [128, 1], F32)
        P.op("pool", lambda e: e.memset(eps_r[:], RMS_EPS), writes=["eps_r"])
        c["tp_b"] = Pool(K, "tpb", 2, [128, 8, 128], BF16, psum=True)
        c["tp_i"] = 0
        hin = Pool(K, "hin", 2, [128, 8, 512], BF16)
        yin = Pool(K, "yin", 2, [128, 2048], F32)
        pz = Pool(K, "pz", 3, [128, 512], F32, psum=True)
        sz = Pool(K, "sz", 2, [128, 512], F32)
        yg = Pool(K, "yg", 2, [128, 2048], F32)
        junk = K.sb("junk", [128, 2048], F32)
        ss = Pool(K, "ss", 2, [128, 16], F32)
        ynb = Pool(K, "ynb", 2, [128, 2048], BF16)
        yTs = Pool(K, "yTs", 2, [128, 16, 512], BF16)
        hTv = hT_d.rearrange("(k p) t -> p k t", p=128)
        ynTv = ynT_d.rearrange("(k p) t -> p k t", p=128)
        for ch in range(8):
            hi_, hik = hin.next()
            P.dma("sp", hi_[:], hTv[:, :, ch * 512:(ch + 1) * 512], reads=hT_key(ch), writes=[hik], stream="ld")
            ys, ysk = yTs.next()
            for t in range(4):
                if t == 0:
                    P.begin_group()
                P.next_stream()
                ti = ch * 4 + t
                y_, yk = yin.next()
                P.dma("sp", y_[:], y_d[ti * 128:(ti + 1) * 128, :], reads=[("y_d", ti)], writes=[yk], stream="ld")
                g_, gk = yg.next()
                for blk in range(4):
                    bs = slice(blk * 512, (blk + 1) * 512)
                    pz_, pzk = pz.next()
                    for k in range(8):
                        P.op("pe", lambda e, pz_=pz_, hi_=hi_, k=k, t=t, bs=bs: e.matmul(
                            pz_[:], hi_[:, k, t * 128:(t + 1) * 128], wz[:, k, bs], start=(k == 0), stop=(k == 7)),
                            reads=[hik, ("wz", k)], writes=[pzk])
                    s_, sk = sz.next()
                    P.op("act", lambda e, s_=s_, pz_=pz_: e.activation(s_[:], pz_[:], AF.Silu), reads=[pzk], writes=[sk])
                    P.op("dve", lambda e, g_=g_, y_=y_, s_=s_, bs=bs: e.tensor_tensor(g_[:, bs], y_[:, bs], s_[:], ALU.mult),
                         reads=[yk, sk], writes=[(gk, blk)])
                q_, qk = ss.next()
                P.op("act", lambda e, g_=g_: e.activation(junk[:], g_[:], AF.Square),
                     reads=[(gk, b) for b in range(4)], writes=["junk"])
                P.op("dve", lambda e, q_=q_: e.tensor_reduce(q_[:, 0:8], junk[:].rearrange("p (g d) -> p g d", d=256), AX.X, ALU.add),
                     reads=["junk"], writes=[(qk, g) for g in range(8)])
                qall = [(qk, g) for g in range(8)]
                P.op("act", lambda e, q_=q_: e.activation(q_[:, 8:16], q_[:, 0:8], AF.Sqrt, bias=eps_r[:], scale=1.0 / 256.0),
                     reads=qall + ["eps_r"], writes=[(qk, "s")])
                P.op("dve", lambda e, q_=q_: e.reciprocal(q_[:, 8:16], q_[:, 8:16]), reads=[(qk, "s")], writes=[(qk, "s")])
                P.op("dve", lambda e, g_=g_, q_=q_: e.tensor_tensor(
                    g_[:].rearrange("p (g d) -> p g d", d=256), g_[:].rearrange("p (g d) -> p g d", d=256),
                    bc_last(q_[:, 8:16], 256), ALU.mult),
                    reads=[(gk, b) for b in range(4)] + [(qk, "s")], writes=[(gk, b) for b in range(4)])
                nb, nbk = ynb.next()
                P.op("dve", lambda e, nb=nb, g_=g_: e.tensor_tensor(nb[:], g_[:], ng[:], ALU.mult),
                     reads=[(gk, b) for b in range(4)] + ["ng"], writes=[nbk])
                transpose_to_hT(K, c, nb, nbk, ys, (ysk, t), 16, t * 128)
                if t == 3:
                    P.end_group()
            for k0 in (0, 8):
                P.dma("pool", ynTv[:, k0:k0 + 8, ch * 512:(ch + 1) * 512], ys[:, k0:k0 + 8, :], reads=[(ysk, t) for t in range(4)],
                      writes=[("ynT_d", ch, k0)], stream="st")


def phase_router(K, c, h1_d, h1_key, router, gates):
    P = K.P
    with K.scope():
        rt = K.sb("rt", [128, 8, 8], F32)
        P.dma("sp", rt[:], router.rearrange("(k p) e -> p k e", p=128), writes=["rt"], stream="cst")
        hin = Pool(K, "rhin", 4, [128, D], F32)
        tpf = Pool(K, "tpf", 4, [128, 4, 128], F32, psum=True)
        hTf = Pool(K, "hTf", 4, [128, 8, 128], F32)
        plg = Pool(K, "plg", 4, [128, 512], F32, psum=True)
        sm = Pool(K, "rsm", 4, [128, 8, 8], F32)
        for ti in range(32):
            if ti % 4 == 0:
                P.begin_group()
            P.next_stream()
            h_, hk = hin.next()
            P.dma("sp", h_[:], h1_d[ti * 128:(ti + 1) * 128, :], reads=h1_key(ti), writes=[hk], stream="ld")
            hT_, hTk = hTf.next()
            for half in range(2):
                pt, ptk = tpf.next()
                for k in range(4):
                    kk = half * 4 + k
                    P.op("pe", lambda e, pt=pt, k=k, kk=kk, h_=h_: e.transpose(pt[:, k, :], h_[:, kk * 128:(kk + 1) * 128], c["ident_f"][:]),
                         reads=[hk, "ident_f"], writes=[ptk])
                if half == 0:
                    P.op("act", lambda e, hT_=hT_, pt=pt: e.copy(hT_[:, 0:4, :], pt[:]), reads=[ptk], writes=[(hTk, 0)])
                else:
                    P.op("dve", lambda e, hT_=hT_, pt=pt: e.tensor_copy(hT_[:, 4:8, :], pt[:]), reads=[ptk], writes=[(hTk, 1)])
            pl, plk = plg.next()
            for k in range(8):
                P.op("pe", lambda e, pl=pl, hT_=hT_, k=k: e.matmul(pl[:, 0:8], hT_[:, k, :], rt[:, k, :], start=(k == 0), stop=(k == 7)),
                     reads=[(hTk, k // 4), "rt"], writes=[plk])
            s_, sk = sm.next()
            lg, m8, ex, msk, den = (s_[:, q, :] for q in range(5))
            P.op("act", lambda e, lg=lg, pl=pl: e.copy(lg, pl[:, 0:8]), reads=[plk], writes=[(sk, "lg")])
            P.op("dve", lambda e, m8=m8, lg=lg: e.max(m8, lg), reads=[(sk, "lg")], writes=[(sk, "m8")])
            P.op("dve", lambda e, den=den, m8=m8: e.tensor_scalar(den[:, 1:2], m8[:, 0:1], -1.0, None, ALU.mult),
                 reads=[(sk, "m8")], writes=[(sk, "nm")])
            P.op("act", lambda e, ex=ex, lg=lg, den=den: e.activation(ex, lg, AF.Exp, bias=den[:, 1:2], scale=1.0),
                 reads=[(sk, "lg"), (sk, "nm")], writes=[(sk, "ex")])
            P.op("dve", lambda e, msk=msk, lg=lg, m8=m8: e.tensor_scalar(msk, lg, m8[:, 1:2], None, ALU.is_ge),
                 reads=[(sk, "lg"), (sk, "m8")], writes=[(sk, "msk")])
            P.op("dve", lambda e, ex=ex, msk=msk: e.tensor_tensor(ex, ex, msk, ALU.mult),
                 reads=[(sk, "ex"), (sk, "msk")], writes=[(sk, "ex")])
            P.op("dve", lambda e, den=den, ex=ex: e.reduce_sum(den[:, 0:1], ex, AX.X), reads=[(sk, "ex")], writes=[(sk, "den")])
            P.op("dve", lambda e, den=den: e.reciprocal(den[:, 2:3], den[:, 0:1]), reads=[(sk, "den")], writes=[(sk, "rden")])
            P.op("dve", lambda e, ex=ex, den=den, ti=ti: e.tensor_scalar(gates[:, ti, :], ex, den[:, 2:3], None, ALU.mult),
                 reads=[(sk, "ex"), (sk, "rden")], writes=[("gates", ti)])
            if ti % 4 == 3:
                P.end_group()


def phase_moe(K, c, h1T_d, h1T_key, w_g, w_u, w_d, gates, ffn_d):
    P = K.P
    TB = 2048
    NTB = TB // 128
    JG = 4
    NG = EDIM // (128 * JG)
    with K.scope():
        hin = Pool(K, "mhin", 1, [128, 8, TB], BF16)
        acc = K.sb("macc", [128, NTB, D], F32)
        wg = Pool(K, "mwg", 2, [128, 8, 512], BF16)
        wu = Pool(K, "mwu", 2, [128, 8, 512], BF16)
        wd = Pool(K, "mwd", 2, [128, JG, D], BF16)
        actT = Pool(K, "mact", 1, [128, JG, TB], BF16)
        sgt = Pool(K, "msg", 2, [128, 512], F32)
        pgu = Pool(K, "mpgu", 2, [128, 2, 512], F32, psum=True)
        pdn = Pool(K, "mpdn", 2, [128, 2, 512], F32, psum=True)
        h1Tv = h1T_d.rearrange("(k p) t -> p k t", p=128)
        for blk in range(S // TB):
            hi_, hik = hin.next()
            for q in range(TB // 512):
                P.dma("sp", hi_[:, :, q * 512:(q + 1) * 512], h1Tv[:, :, blk * TB + q * 512: blk * TB + (q + 1) * 512],
                      reads=h1T_key((blk * TB) // 512 + q), writes=[(hik, q)], stream="ld")
            first = True
            for ex in range(NE):
                wgr = w_g[ex].rearrange("(k p) c -> p k c", p=128)
                wur = w_u[ex].rearrange("(k p) c -> p k c", p=128)
                wdr = w_d[ex].rearrange("(j p) c -> p j c", p=128)
                for jg in range(NG):
                    wg_, wgk = wg.next()
                    wu_, wuk = wu.next()
                    wd_, wdk = wd.next()
                    cs = slice(jg * 512, (jg + 1) * 512)
                    for k0 in (0, 4):
                        P.dma("pool", wg_[:, k0:k0 + 4, :], wgr[:, k0:k0 + 4, cs], writes=[(wgk, k0)], stream="wc")
                        P.dma("pool", wu_[:, k0:k0 + 4, :], wur[:, k0:k0 + 4, cs], writes=[(wuk, k0)], stream="wc")
                    P.dma("pool", wd_[:], wdr[:, jg * JG:(jg + 1) * JG, :], writes=[wdk], stream="wc")
                    at, atk = actT.next()
                    for q in range(TB // 512):
                        qs = slice(q * 512, (q + 1) * 512)
                        for jj in range(JG):
                            pg_, pgk_ = pgu.next()
                            for gu, (w_, wk_) in enumerate(((wg_, wgk), (wu_, wuk))):
                                for k in range(8):
                                    P.op("pe", lambda e, pg_=pg_, w_=w_, gu=gu, k=k, jj=jj, hi_=hi_, qs=qs: e.matmul(
                                        pg_[:, gu, :], w_[:, k, jj * 128:(jj + 1) * 128], hi_[:, k, qs],
                                        start=(k == 0), stop=(k == 7)),
                                        reads=[(wk_, (k // 4) * 4), (hik, q)], writes=[(pgk_, gu)])
                            sg_, sgk_ = sgt.next()
                            P.op("act", lambda e, sg_=sg_, pg_=pg_: e.activation(sg_[:], pg_[:, 0, :], AF.Silu),
                                 reads=[(pgk_, 0)], writes=[sgk_])
                            P.op("dve", lambda e, at=at, jj=jj, qs=qs, sg_=sg_, pg_=pg_: e.tensor_tensor(
                                at[:, jj, qs], sg_[:], pg_[:, 1, :], ALU.mult),
                                reads=[sgk_, (pgk_, 1)], writes=[(atk, jj, q)])
                    for t in range(NTB):
                        ti = blk * NTB + t
                        pd_, pdk = pdn.next()
                        for half in range(2):
                            for jj in range(JG):
                                P.op("pe", lambda e, pd_=pd_, at=at, jj=jj, t=t, half=half, wd_=wd_: e.matmul(
                                    pd_[:, half, :], at[:, jj, t * 128:(t + 1) * 128], wd_[:, jj, half * 512:(half + 1) * 512],
                                    start=(jj == 0), stop=(jj == JG - 1)),
                                    reads=[(atk, jj, t // 4), wdk], writes=[(pdk, half)])
                        src = pd_[:].rearrange("p a b -> p (a b)")
                        if first:
                            P.op("dve", lambda e, t=t, src=src, ti=ti, ex=ex: e.tensor_scalar(
                                acc[:, t, :], src, gates[:, ti, ex:ex + 1], None, ALU.mult),
                                reads=[(pdk, 0), (pdk, 1), ("gates", ti)], writes=[("macc", t)])
                        else:
                            P.op("dve", lambda e, t=t, src=src, ti=ti, ex=ex: e.scalar_tensor_tensor(
                                acc[:, t, :], src, gates[:, ti, ex:ex + 1], acc[:, t, :], ALU.mult, ALU.add),
                                reads=[(pdk, 0), (pdk, 1), ("gates", ti), ("macc", t)], writes=[("macc", t)])
                    first = False
            for t in range(NTB):
                ti = blk * NTB + t
                P.dma("sp", ffn_d[ti * 128:(ti + 1) * 128, :], acc[:, t, :], reads=[("macc", t)], writes=[("ffn_d", ti)], stream="st")


def phase_epilogue(K, c, h1_d, h1_key, ffn_d, ffn_key, g_row, b_row, w_pg, w_pp, p_d, out_d):
    P = K.P
    with K.scope():
        g_bc, b_bc, wpg, wpp = epilogue_setup(K, c, g_row, b_row, w_pg, w_pp, npg=2, nb=4)
        h1in = Pool(K, "h1in", 4, [128, D], F32)
        fin = Pool(K, "fin", 4, [128, D], F32)
        zt = Pool(K, "zt", 4, [128, D], F32)
        ctxs = {}

        def s1(ti):
            ts = slice(ti * 128, (ti + 1) * 128)
            h_, hk = h1in.next()
            f_, fk = fin.next()
            P.dma("sp", h_[:], h1_d[ts, :], reads=h1_key(ti), writes=[hk], stream="ld")
            P.dma("sp", f_[:], ffn_d[ts, :], reads=ffn_key(ti), writes=[fk], stream="ld")
            z, zk = zt.next()
            P.op("dve", lambda e: e.scalar_tensor_tensor(z[:], h_[:], ALPHA, f_[:], ALU.mult, ALU.add),
                 reads=[hk, fk], writes=[zk])
            ctxs[ti] = epilogue_s1(K, c, z, zk, ti * 128, g_bc, b_bc, p_d)

        for g0 in range(0, 32, 4):
            P.begin_group()
            for ti in range(g0, g0 + 4):
                P.next_stream()
                s1(ti)
                epilogue_s2(K, c, ctxs.pop(ti), wpg, wpp, out_d, "out_d", None)
            P.end_group()


def build(mode="full", upto=99):
    K = KB()
    nc = K.nc
    P = K.P
    ein = lambda name, shape: nc.dram_tensor(name, shape, F32, kind="ExternalInput").ap()
    p_d = ein("p", [2, S, 256])
    ln_mix_g = ein("ln_mix_g", [2, D]); ln_mix_b = ein("ln_mix_b", [2, D])
    ln_ffn_g = ein("ln_ffn_g", [2, D]); ln_ffn_b = ein("ln_ffn_b", [2, D])
    ple_w_proj = ein("ple_w_proj", [2, 256, D]); ple_w_gate = ein("ple_w_gate", [2, D, D])
    c = make_consts(K)
    c["eps_ln"] = K.sb("eps_ln", [128, 1], F32)
    P.op("pool", lambda e: e.memset(c["eps_ln"][:], LN_EPS), writes=["eps_ln"])
    c["tp_eng"] = ("act", "dve")
    c["tp_i"] = 0
    if mode in ("l0", "full"):
        x_d = ein("x", [S, D])
        fox_w_in = ein("fox_w_in", [D, 3088]); fox_b_f = ein("fox_b_f", [16]); fox_w_o = ein("fox_w_o", [D, D])
        ffn_w_gate = ein("ffn_w_gate", [D, FFN]); ffn_w_up = ein("ffn_w_up", [D, FFN]); ffn_w_down = ein("ffn_w_down", [FFN, D])
        qaT = K.dram("qaT", [16, 66, S], BF16)
        kaT = K.dram("kaT", [16, 66, S], BF16)
        vaug = K.dram("vaug", [S, 16, 128], BF16)
        attnT = K.dram("attnT", [D, S], BF16)
        h1 = K.dram("l0_h1", [S, D], F32)
        h1T = K.dram("l0_h1T", [D, S], BF16)
        ctok = K.sb("ctok", [128, 32, 16], F32)
        cref = K.sb("cref", [128, 8, 16], F32)
        if mode == "l0":
            h3_d = nc.dram_tensor("out", [S, D], F32, kind="ExternalOutput").ap()
            h3T_d = nc.dram_tensor("outT", [D, S], BF16, kind="ExternalOutput").ap()
        else:
            h3_d = K.dram("l0_h3", [S, D], F32)
            h3T_d = K.dram("l0_h3T", [D, S], BF16)
        phase_qkv(K, c, x_d, fox_w_in, fox_b_f, qaT, kaT, vaug, ctok, cref)
        phase_attn(K, c, qaT, kaT, vaug, ctok, cref, attnT)
        phase_proj_ln(K, c, attnT, lambda ch: [("attn_d", ch)], 8, fox_w_o, x_d, lambda t: [],
                      ln_mix_g[0], ln_mix_b[0], h1, h1T, "l0")
        actT_d = K.dram("l0_actT", [FFN, S], BF16)
        phase_ffn(K, c, h1, h1T, ffn_w_gate, ffn_w_up, ffn_w_down, ln_ffn_g[0], ln_ffn_b[0],
                  ple_w_gate[0], ple_w_proj[0], p_d[0], h3_d, h3T_d, actT_d)
        h3_key = lambda ti: [("out_d", ti)]
        h3T_key = lambda ch: [("outT_d", ch)]
    if mode == "l1":
        h3_d = ein("h", [S, D])
        h3T_d = nc.dram_tensor("hT", [D, S], BF16, kind="ExternalInput").ap()
        h3_key = lambda ti: []
        h3T_key = lambda ch: []
    if mode in ("l1", "full"):
        ssd_w_in = ein("ssd_w_in", [D, 6176]); ssd_conv_w = ein("ssd_conv_w", [4, 4096]); ssd_conv_b = ein("ssd_conv_b", [4096])
        ssd_dt_bias = ein("ssd_dt_bias", [32]); ssd_a_log = ein("ssd_a_log", [32]); ssd_d = ein("ssd_d", [32])
        ssd_norm_g = ein("ssd_norm_g", [2048]); ssd_w_out = ein("ssd_w_out", [2048, D])
        moe_router = ein("moe_router", [D, NE]); moe_w_gate = ein("moe_w_gate", [NE, D, EDIM])
        moe_w_up = ein("moe_w_up", [NE, D, EDIM]); moe_w_down = ein("moe_w_down", [NE, EDIM, D])
        out_d = nc.dram_tensor("out", [S, D], F32, kind="ExternalOutput").ap()
        xs_d = K.dram("xs_d", [S, 2048], F32)
        Btok_d = K.dram("Btok_d", [S, 1024], BF16)
        BT_d = K.dram("BT_d", [8, 128, S], BF16)
        CT_d = K.dram("CT_d", [8, 128, S], BF16)
        y_d = K.dram("y_d", [S, 2048], F32)
        ynT_d = K.dram("ynT_d", [2048, S], BF16)
        g1 = K.dram("l1_h1", [S, D], F32)
        g1T = K.dram("l1_h1T", [D, S], BF16)
        ffn_d = K.dram("ffn_d", [S, D], F32)
        dt_tok = K.sb("dt_tok", [128, 32, 32], F32)
        gates = K.sb("gates", [128, 32, NE], F32)
        phase_ssd_proj(K, c, h3T_d, h3T_key, ssd_w_in, ssd_conv_w, ssd_conv_b, ssd_dt_bias, xs_d, Btok_d, BT_d, CT_d, dt_tok)
        if upto >= 2:
            phase_ssd_core(K, c, xs_d, Btok_d, BT_d, CT_d, dt_tok, ssd_a_log, ssd_d, y_d)
        if upto >= 3:
            phase_ssd_gate(K, c, y_d, h3T_d, h3T_key, ssd_w_in, ssd_norm_g, ynT_d)
        if upto >= 4:
            phase_proj_ln(K, c, ynT_d, lambda ch: [("ynT_d", ch, 0), ("ynT_d", ch, 8)], 16, ssd_w_out, h3_d, h3_key,
                          ln_mix_g[1], ln_mix_b[1], g1, g1T, "l1")
        if upto >= 5:
            phase_router(K, c, g1, lambda ti: [("l1h1_d", ti)], moe_router, gates)
        if upto >= 6:
            phase_moe(K, c, g1T, lambda ch: [("l1h1T_d", ch)], moe_w_gate, moe_w_up, moe_w_down, gates, ffn_d)
        if upto >= 7:
            phase_epilogue(K, c, g1, lambda ti: [("l1h1_d", ti)], ffn_d, lambda ti: [("ffn_d", ti)],
                           ln_ffn_g[1], ln_ffn_b[1], ple_w_gate[1], ple_w_proj[1], p_d[1], out_d)
        if upto < 7:
            dbg = {1: xs_d, 2: y_d, 3: None, 4: g1, 5: g1, 6: ffn_d}[upto]
            if dbg is not None:
                dbg_o = nc.dram_tensor("dbg", list(dbg.shape), F32, kind="ExternalOutput").ap()
                P.dma("sp", dbg_o, dbg, reads=[k for k in P.lastw if isinstance(k, tuple) and k[0] in ("xs_d", "y_d", "l1h1_d", "ffn_d")],
                      writes=["dbg_o"], stream="st")
    P.flush(barrier=True)
    K.root.close()
    return nc, P


FUSED = True

_COMMON = ["ln_mix_g", "ln_mix_b", "ln_ffn_g", "ln_ffn_b", "ple_w_proj", "ple_w_gate"]
_L0 = ["fox_w_in", "fox_b_f", "fox_w_o", "ffn_w_gate", "ffn_w_up", "ffn_w_down"]
_L1 = ["ssd_w_in", "ssd_conv_w", "ssd_conv_b", "ssd_dt_bias", "ssd_a_log", "ssd_d", "ssd_norm_g", "ssd_w_out",
       "moe_router", "moe_w_gate", "moe_w_up", "moe_w_down"]


def kernel(**inputs):
    f = lambda a: np.ascontiguousarray(np.asarray(a, dtype=np.float32))
    x = f(inputs["x"])
    p = f(inputs["p"])
    B = x.shape[0]
    common = {k: f(inputs[k]) for k in _COMMON}
    l0 = {k: f(inputs[k])[0] for k in _L0}
    l1 = {k: f(inputs[k])[0] for k in _L1}
    cores = list(range(B))
    if FUSED:
        nc, _ = build("full")
        maps = [dict(common, **l0, **l1, x=x[b], p=np.ascontiguousarray(p[:, b])) for b in cores]
        res = run_bass_kernel_spmd(nc, maps, core_ids=cores)
        return np.stack([np.asarray(r["out"], dtype=np.float32) for r in res.results], axis=0)
    nc0, _ = build("l0")
    maps = [dict(common, **l0, x=x[b], p=np.ascontiguousarray(p[:, b])) for b in cores]
    r0 = run_bass_kernel_spmd(nc0, maps, core_ids=cores).results
    nc1, _ = build("l1")
    maps = [dict(common, **l1, h=r0[b]["out"], hT=r0[b]["outT"], p=np.ascontiguousarray(p[:, b])) for b in cores]
    r1 = run_bass_kernel_spmd(nc1, maps, core_ids=cores).results
    return np.stack([np.asarray(r["out"], dtype=np.float32) for r in r1], axis=0)
```
